# Optimizing a Trainium2 kernel written in Bass

```python
import jax
import jax.numpy as jnp
from jax import lax
import numpy as np

D_MODEL = 2048
BATCH = 2
SEQ = 8192
DEPTH = 2

GRID_W = 64
CTX_LEN = 256

A_WIDTH = 1024
A_HEADS = 8
A_HEAD = A_WIDTH // A_HEADS
A_CHUNK = 64
B_WIDTH = 1024
B_HEADS = 16
B_HEAD = B_WIDTH // B_HEADS
W_RANK = 64
A_RANK = 64
G_RANK = 160
N_EXPERTS = 32
TOP_K = 4
D_EXPERT = 2048
SWIGLU_ALPHA = 1.702
SWIGLU_LIMIT = 7.0

RMS_EPS = 1e-6
GN_EPS = 64e-5

A_COLS = 5 * A_WIDTH
B_COLS = 3 * B_WIDTH + 2 * W_RANK + 2 * A_RANK + G_RANK
GATE_COLS = 2 * D_MODEL
IN_COLS = A_COLS + B_COLS + GATE_COLS
B_SPLITS = (B_WIDTH, 2 * B_WIDTH, 3 * B_WIDTH, 3 * B_WIDTH + 2 * W_RANK,
            3 * B_WIDTH + 2 * W_RANK + 2 * A_RANK)

kernel_name = 'hybrid_hgrn2_rwkv7_moe_prefix_dit'


def rmsnorm(x, g):
    xf = x.astype(jnp.float32)
    y = xf * lax.rsqrt(jnp.mean(xf * xf, axis=-1, keepdims=True) + RMS_EPS)
    return (y * g.astype(jnp.float32)).astype(x.dtype)


def modulate(h, shift, scale):
    return h * (1.0 + scale) + shift


def flip(t):
    return jnp.flip(t, axis=1)


def split_heads(t, n_heads):
    return t.reshape(t.shape[:-1] + (n_heads, t.shape[-1] // n_heads))


def merge_heads(t):
    return t.reshape(t.shape[:-2] + (-1,))


def grid_qshift(t, rows):
    bsz, L, C = t.shape
    g = t.reshape(bsz, rows, GRID_W, C // 4, 4)
    left = jnp.pad(g[:, :, :-1, :, 0], ((0, 0), (0, 0), (1, 0), (0, 0)))
    right = jnp.pad(g[:, :, 1:, :, 1], ((0, 0), (0, 0), (0, 1), (0, 0)))
    up = jnp.pad(g[:, :-1, :, :, 2], ((0, 0), (1, 0), (0, 0), (0, 0)))
    down = jnp.pad(g[:, 1:, :, :, 3], ((0, 0), (0, 1), (0, 0), (0, 0)))
    return jnp.stack([left, right, up, down], axis=-1).reshape(bsz, L, C)


def seq_bishift(t):
    bsz, L, C = t.shape
    g = t.reshape(bsz, L, C // 2, 2)
    prev = jnp.pad(g[:, :-1, :, 0], ((0, 0), (1, 0), (0, 0)))
    nxt = jnp.pad(g[:, 1:, :, 1], ((0, 0), (0, 1), (0, 0)))
    return jnp.stack([prev, nxt], axis=-1).reshape(bsz, L, C)


def hgrn2_chunk_scan(q, k, v, log_f, state0):
    bsz, L, H, _ = q.shape
    n_chunks = L // A_CHUNK

    def chunks(t):
        t = t.astype(jnp.float32)
        return jnp.moveaxis(t.reshape(bsz, n_chunks, A_CHUNK, H, t.shape[-1]), 1, 0)

    incl = jnp.tril(jnp.ones((A_CHUNK, A_CHUNK), dtype=bool))[None, :, :, None, None]

    def step(S, inp):
        qc, kc, vc, gc = inp
        b = jnp.cumsum(gc, axis=1)
        decay = jnp.exp(jnp.where(incl, b[:, :, None] - b[:, None, :], -jnp.inf))
        scores = jnp.einsum('bthk,btshk,bshk->bhts', qc, decay, kc)
        o = (jnp.einsum('bhts,bshv->bthv', scores, vc)
             + jnp.einsum('bthk,bhkv->bthv', qc * jnp.exp(b), S))
        b_end = b[:, -1]
        S = (jnp.exp(b_end)[..., None] * S
             + jnp.einsum('bshk,bshv->bhkv', kc * jnp.exp(b_end[:, None] - b), vc))
        return S, o

    S_end, o = lax.scan(step, state0, (chunks(q), chunks(k), chunks(v), chunks(log_f)))
    return jnp.moveaxis(o, 0, 1).reshape(bsz, L, H, -1).astype(v.dtype), S_end


def rwkv7_scan(r, w, k, v, kk, a, state0):
    def step(S, inp):
        r_t, w_t, k_t, v_t, kk_t, a_t = inp
        S = (S * w_t[:, :, None, :]
             - jnp.einsum('bhvk,bhk->bhv', S, kk_t)[..., None] * (kk_t * a_t)[:, :, None, :]
             + v_t[..., None] * k_t[:, :, None, :])
        return S, jnp.einsum('bhvk,bhk->bhv', S, r_t)

    xs = tuple(jnp.moveaxis(t.astype(jnp.float32), 1, 0) for t in (r, w, k, v, kk, a))
    S_end, o = lax.scan(step, state0, xs)
    return jnp.moveaxis(o, 0, 1).astype(v.dtype), S_end


def bidir_prefix_scan(scan_fn, ctx_fwd, ctx_bwd, lat_fwd, lat_bwd, state0):
    oc_f, s_f = scan_fn(*ctx_fwd, state0)
    oc_b, s_b = scan_fn(*(flip(t) for t in ctx_bwd), state0)
    ol_f, _ = scan_fn(*lat_fwd, s_f)
    ol_b, _ = scan_fn(*(flip(t) for t in lat_bwd), s_b)
    return oc_f + flip(oc_b), ol_f + flip(ol_b)


def hgrn2_features(pa, lb):
    q, pf_f, pf_b, i, og = jnp.split(pa, 5, axis=-1)

    def gates(pf, lbd):
        pf = pf.astype(jnp.float32)
        k = (1.0 - lbd) * jax.nn.sigmoid(-pf)
        log_f = jnp.logaddexp(jnp.log(lbd), jnp.log1p(-lbd) + jax.nn.log_sigmoid(pf))
        return split_heads(k, A_HEADS), split_heads(log_f, A_HEADS)

    k_f, lf_f = gates(pf_f, lb[0])
    k_b, lf_b = gates(pf_b, lb[1])
    q = split_heads(jax.nn.silu(q), A_HEADS)
    i = split_heads(i, A_HEADS)
    return (q, k_f, i, lf_f), (q, k_b, i, lf_b), og


def hgrn2_readout(o, og, g):
    of = o.astype(jnp.float32)
    of = of * lax.rsqrt(jnp.mean(of * of, axis=-1, keepdims=True) + RMS_EPS)
    return (merge_heads(of) * g).astype(og.dtype) * jax.nn.silu(og)


def rwkv7_features(pb, shifted, mu, w0, w_up, a0, a_up, g_up, k_k, k_a):
    m = pb + mu * (shifted - pb)
    r, k, v, wd, ad, gd = jnp.split(m, B_SPLITS, axis=-1)
    lead = pb.shape[:2]
    wd = wd.reshape(lead + (2, W_RANK))
    ad = ad.reshape(lead + (2, A_RANK))
    w_log = -jax.nn.softplus(-(jnp.einsum('bldr,drc->bldc', jnp.tanh(wd), w_up) + w0)) - 0.5
    decay = jnp.exp(-jnp.exp(w_log.astype(jnp.float32)))
    a = jax.nn.sigmoid(jnp.einsum('bldr,drc->bldc', ad, a_up) + a0)
    g = jax.nn.sigmoid(gd) @ g_up
    kk = split_heads(k * k_k, B_HEADS)
    kk_norm = jnp.sqrt(jnp.sum(jnp.square(kk.astype(jnp.float32)), axis=-1, keepdims=True))
    kk = kk / jnp.maximum(kk_norm, 1e-12).astype(kk.dtype)
    k_dir = k[:, :, None] * (1.0 + (a - 1.0) * k_a)
    h = lambda t: split_heads(t, B_HEADS)
    r, v = h(r), h(v)
    fwd = (r, h(decay[:, :, 0]), h(k_dir[:, :, 0]), v, kk, h(a[:, :, 0]))
    bwd = (r, h(decay[:, :, 1]), h(k_dir[:, :, 1]), v, kk, h(a[:, :, 1]))
    return fwd, bwd, (r, h(k_dir[:, :, 0] + k_dir[:, :, 1]), v, g)


def rwkv7_readout(o, r, k_sum, v, g, r_k, ln_w, ln_b):
    of = o.astype(jnp.float32)
    mean = jnp.mean(of, axis=-1, keepdims=True)
    var = jnp.mean(jnp.square(of - mean), axis=-1, keepdims=True)
    y = merge_heads((of - mean) * lax.rsqrt(var + GN_EPS)) * ln_w + ln_b
    bonus = jnp.sum(r * k_sum * r_k, axis=-1, keepdims=True) * v
    return (y.astype(v.dtype) + merge_heads(bonus)) * g


def token_mixer(hc, hl, rows, lb, p, need_ctx):
    pc = hc @ p['w_in']
    pl = hl @ p['w_in']
    pa_c, pb_c, pg_c = jnp.split(pc, (A_COLS, A_COLS + B_COLS), axis=-1)
    pa_l, pb_l, pg_l = jnp.split(pl, (A_COLS, A_COLS + B_COLS), axis=-1)
    bsz = hl.shape[0]

    a_fc, a_bc, og_c = hgrn2_features(pa_c, lb)
    a_fl, a_bl, og_l = hgrn2_features(pa_l, lb)
    z_a = jnp.zeros((bsz, A_HEADS, A_HEAD, A_HEAD), jnp.float32)
    oa_c, oa_l = bidir_prefix_scan(hgrn2_chunk_scan, a_fc, a_bc, a_fl, a_bl, z_a)

    rw = (p['rwkv_mu'], p['rwkv_w0'], p['rwkv_w_up'], p['rwkv_a0'], p['rwkv_a_up'],
          p['rwkv_g_up'], p['rwkv_k_k'], p['rwkv_k_a'])
    b_fc, b_bc, ex_c = rwkv7_features(pb_c, seq_bishift(pb_c), *rw)
    b_fl, b_bl, ex_l = rwkv7_features(pb_l, grid_qshift(pb_l, rows), *rw)
    z_b = jnp.zeros((bsz, B_HEADS, B_HEAD, B_HEAD), jnp.float32)
    ob_c, ob_l = bidir_prefix_scan(rwkv7_scan, b_fc, b_bc, b_fl, b_bl, z_b)

    def merge(oa, og, ob, ex, pg):
        ya = hgrn2_readout(oa, og, p['hgrn_norm_g'])
        yb = rwkv7_readout(ob, *ex, p['rwkv_r_k'], p['rwkv_ln_w'], p['rwkv_ln_b'])
        g_a, g_b = jnp.split(jax.nn.sigmoid(pg), 2, axis=-1)
        return (g_a * (ya @ p['w_branch_a']) + g_b * (yb @ p['w_branch_b'])) @ p['w_out']

    y_l = merge(oa_l, og_l, ob_l, ex_l, pg_l)
    y_c = merge(oa_c, og_c, ob_c, ex_c, pg_c) if need_ctx else None
    return y_c, y_l


def clamped_swiglu(h):
    x_glu = jnp.minimum(h[..., ::2], SWIGLU_LIMIT)
    x_lin = jnp.clip(h[..., 1::2], -SWIGLU_LIMIT, SWIGLU_LIMIT)
    return x_glu * jax.nn.sigmoid(SWIGLU_ALPHA * x_glu) * (x_lin + 1.0)


def moe_ffn(h, w_r, b_r, w1, b1, w2, b2):
    lead = h.shape[:-1]
    t = h.reshape(-1, D_MODEL)
    logits = (t @ w_r + b_r).astype(jnp.float32)
    top_val, top_idx = lax.top_k(logits, TOP_K)
    weights = jax.nn.softmax(top_val, axis=-1)
    combine = jnp.einsum('nk,nke->ne', weights,
                         jax.nn.one_hot(top_idx, N_EXPERTS, dtype=jnp.float32)).astype(h.dtype)
    out = jnp.zeros_like(t)
    for e in range(N_EXPERTS):
        y = clamped_swiglu(t @ w1[e] + b1[e]) @ w2[e] + b2[e]
        out = out + combine[:, e:e + 1] * y
    return out.reshape(lead + (D_MODEL,))


def setup_inputs(seed: int = 0) -> dict:
    key = jax.random.key(seed)
    ks = iter(jax.random.split(key, 40))
    f32 = jnp.float32
    D = D_MODEL

    def nrm(shape, scale):
        return jax.random.normal(next(ks), shape, f32) * scale

    return {
        'x': nrm((BATCH, SEQ, D), 1.0),
        'c': nrm((BATCH, D), 1.0),
        'ctx': nrm((BATCH, CTX_LEN, D), 1.0),
        'c_ctx': nrm((D,), 1.0),
        'ada_w': nrm((DEPTH, D, 6 * D), 0.5 * D ** -0.5),
        'ada_b': nrm((DEPTH, 6 * D), 0.02),
        'norm_mix_g': 1.0 + nrm((DEPTH, D), 0.02),
        'norm_ffn_g': 1.0 + nrm((DEPTH, D), 0.02),
        'final_norm_g': 1.0 + nrm((D,), 0.02),
        'w_in': nrm((DEPTH, D, IN_COLS), D ** -0.5),
        'hgrn_lb_logits': nrm((DEPTH, 2, A_WIDTH), 0.5),
        'hgrn_norm_g': 1.0 + nrm((DEPTH, A_WIDTH), 0.02),
        'rwkv_mu': jax.random.uniform(next(ks), (DEPTH, B_COLS), f32),
        'rwkv_w0': nrm((DEPTH, 2, B_WIDTH), 0.5),
        'rwkv_w_up': nrm((DEPTH, 2, W_RANK, B_WIDTH), W_RANK ** -0.5),
        'rwkv_a0': nrm((DEPTH, 2, B_WIDTH), 0.5),
        'rwkv_a_up': nrm((DEPTH, 2, A_RANK, B_WIDTH), A_RANK ** -0.5),
        'rwkv_g_up': nrm((DEPTH, G_RANK, B_WIDTH), G_RANK ** -0.5),
        'rwkv_k_k': 0.85 + nrm((DEPTH, B_WIDTH), 0.02),
        'rwkv_k_a': 1.0 + nrm((DEPTH, B_WIDTH), 0.02),
        'rwkv_r_k': nrm((DEPTH, B_HEADS, B_HEAD), 0.1),
        'rwkv_ln_w': 1.0 + nrm((DEPTH, B_WIDTH), 0.02),
        'rwkv_ln_b': nrm((DEPTH, B_WIDTH), 0.02),
        'w_branch_a': nrm((DEPTH, A_WIDTH, D), A_WIDTH ** -0.5),
        'w_branch_b': nrm((DEPTH, B_WIDTH, D), B_WIDTH ** -0.5),
        'w_out': nrm((DEPTH, D, D), D ** -0.5),
        'router_w': nrm((DEPTH, D, N_EXPERTS), D ** -0.5),
        'router_b': nrm((DEPTH, N_EXPERTS), 0.01),
        'expert_w1': nrm((DEPTH, N_EXPERTS, D, 2 * D_EXPERT), D ** -0.5),
        'expert_b1': nrm((DEPTH, N_EXPERTS, 2 * D_EXPERT), 0.01),
        'expert_w2': nrm((DEPTH, N_EXPERTS, D_EXPERT, D), D_EXPERT ** -0.5),
        'expert_b2': nrm((DEPTH, N_EXPERTS, D), 0.01),
    }


def reference(x, c, ctx, c_ctx, ada_w, ada_b, norm_mix_g, norm_ffn_g, final_norm_g, w_in,
              hgrn_lb_logits, hgrn_norm_g, rwkv_mu, rwkv_w0, rwkv_w_up, rwkv_a0, rwkv_a_up,
              rwkv_g_up, rwkv_k_k, rwkv_k_a, rwkv_r_k, rwkv_ln_w, rwkv_ln_b, w_branch_a,
              w_branch_b, w_out, router_w, router_b, expert_w1, expert_b1, expert_w2, expert_b2):
    rows = x.shape[1] // GRID_W
    n_ctx = ctx.shape[1]
    lb_all = jnp.cumsum(jax.nn.softmax(hgrn_lb_logits.astype(jnp.float32), axis=0), axis=0)
    lb_all = lb_all - lb_all[0]
    c_act = jax.nn.silu(c)
    ctx_act = jax.nn.silu(c_ctx)
    xl, xc = x, ctx
    for l in range(DEPTH):
        need_ctx = l < DEPTH - 1
        sh1, sc1, g1, sh2, sc2, g2 = [m[:, None] for m in jnp.split(c_act @ ada_w[l] + ada_b[l], 6, axis=-1)]
        csh1, csc1, cg1, csh2, csc2, cg2 = jnp.split(ctx_act @ ada_w[l] + ada_b[l], 6, axis=-1)
        p = {'w_in': w_in[l], 'hgrn_norm_g': hgrn_norm_g[l], 'rwkv_mu': rwkv_mu[l],
             'rwkv_w0': rwkv_w0[l], 'rwkv_w_up': rwkv_w_up[l], 'rwkv_a0': rwkv_a0[l],
             'rwkv_a_up': rwkv_a_up[l], 'rwkv_g_up': rwkv_g_up[l], 'rwkv_k_k': rwkv_k_k[l],
             'rwkv_k_a': rwkv_k_a[l], 'rwkv_r_k': rwkv_r_k[l], 'rwkv_ln_w': rwkv_ln_w[l],
             'rwkv_ln_b': rwkv_ln_b[l], 'w_branch_a': w_branch_a[l], 'w_branch_b': w_branch_b[l],
             'w_out': w_out[l]}
        hl = modulate(rmsnorm(xl, norm_mix_g[l]), sh1, sc1)
        hc = modulate(rmsnorm(xc, norm_mix_g[l]), csh1, csc1)
        y_c, y_l = token_mixer(hc, hl, rows, lb_all[l], p, need_ctx)
        xl = xl + g1 * y_l
        hl2 = modulate(rmsnorm(xl, norm_ffn_g[l]), sh2, sc2)
        moe_args = (router_w[l], router_b[l], expert_w1[l], expert_b1[l], expert_w2[l], expert_b2[l])
        if need_ctx:
            xc = xc + cg1 * y_c
            hc2 = modulate(rmsnorm(xc, norm_ffn_g[l]), csh2, csc2)
            f = moe_ffn(jnp.concatenate([hc2, hl2], axis=1), *moe_args)
            xc = xc + cg2 * f[:, :n_ctx]
            f_l = f[:, n_ctx:]
        else:
            f_l = moe_ffn(hl2, *moe_args)
        xl = xl + g2 * f_l
    return rmsnorm(xl, final_norm_g)
```

```python
import numpy as np
from contextlib import ExitStack
import concourse.bass as bass
import concourse.mybir as mybir
from concourse.bass_utils import run_bass_kernel_spmd

F32 = mybir.dt.float32
BF16 = mybir.dt.bfloat16
ALU = mybir.AluOpType
AF = mybir.ActivationFunctionType
AX = mybir.AxisListType

NDMA_RING = 8


class T:
    def __init__(self, h, name):
        self.h = h
        self.name = name

    def __getitem__(self, k):
        return self.h[k]


class Prog:
    ENGS = ("pe", "act", "dve", "pool", "sp")

    def __init__(self):
        self.nc = bass.Bass("TRN2", target_bir_lowering=False)
        self.es = ExitStack()
        self.ops = []
        self.nt = 0

    def dram(self, name, shape, dtype=F32, kind="ExternalInput"):
        return self.nc.dram_tensor(name, list(shape), dtype, kind=kind).ap()

    def sb(self, shape, dtype=F32, name=None):
        self.nt += 1
        name = name or f"sb{self.nt}"
        return T(self.es.enter_context(self.nc.sbuf_tensor(name, list(shape), dtype)), name)

    def ps(self, shape, dtype=F32, name=None):
        self.nt += 1
        name = name or f"ps{self.nt}"
        return T(self.es.enter_context(self.nc.psum_tensor(name, list(shape), dtype)), name)

    def op(self, eng, fn, r=(), w=(), dma=False):
        self.ops.append((eng, fn, tuple(r), tuple(w), dma))

    def dma(self, q, out, in_, r=(), w=(), **kw):
        self.op(q, lambda e: e.dma_start(out=out, in_=in_, **kw), r, w, dma=True)

    @staticmethod
    def _cells(key):
        if isinstance(key, tuple):
            return key[0].name, key[1]
        return key.name, None

    def build(self):
        nc = self.nc
        ops = self.ops
        n = len(ops)
        lastw = {}
        readers = {}
        deps = [set() for _ in range(n)]
        for i, (eng, fn, r, w, dma) in enumerate(ops):
            for key in r:
                tn, sub = self._cells(key)
                lw = lastw.setdefault(tn, {})
                if sub is None:
                    for s, j in lw.items():
                        deps[i].add((j, "raw"))
                else:
                    for s in (sub, None):
                        if s in lw:
                            deps[i].add((lw[s], "raw"))
            for key in w:
                tn, sub = self._cells(key)
                lw = lastw.setdefault(tn, {})
                rd = readers.setdefault(tn, {})
                subs = list(set(list(lw.keys()) + list(rd.keys()))) if sub is None else [sub, None]
                for s in subs:
                    if s in lw:
                        deps[i].add((lw[s], "waw"))
                    for j in rd.get(s, ()):
                        if j != i:
                            deps[i].add((j, "war"))
            for key in r:
                tn, sub = self._cells(key)
                readers.setdefault(tn, {}).setdefault(sub, []).append(i)
            for key in w:
                tn, sub = self._cells(key)
                lw = lastw[tn]
                rd = readers.setdefault(tn, {})
                if sub is None:
                    lw.clear()
                    rd.clear()
                else:
                    rd[sub] = []
                lw[sub] = i
        need = [set() for _ in range(n)]
        signal = [False] * n
        for i in range(n):
            ei, dmai = ops[i][0], ops[i][4]
            for (j, kind) in deps[i]:
                ej, dmaj = ops[j][0], ops[j][4]
                if not dmai and not dmaj and ei == ej:
                    if ei == "pe":
                        continue
                need[i].add(j)
                signal[j] = True
        sems = {}
        for e in ("pe", "act", "dve", "pool"):
            sems[e] = self.es.enter_context(nc.semaphore(f"s_{e}"))
        rings = {}
        for q in ("sp", "act", "pool"):
            rings[q] = [self.es.enter_context(nc.semaphore(f"d_{q}{k}")) for k in range(NDMA_RING)]
        sig = [None] * n
        cnt = {e: 0 for e in sems}
        dcnt = {q: 0 for q in rings}
        prevdma = [None] * n
        dma_hist = {q: [] for q in rings}
        for i, (eng, fn, r, w, dma) in enumerate(ops):
            if dma:
                k = dcnt[eng]
                dcnt[eng] += 1
                sig[i] = (rings[eng][k % NDMA_RING], 16 * (k // NDMA_RING + 1))
                if k >= NDMA_RING:
                    prevdma[i] = dma_hist[eng][k - NDMA_RING]
                dma_hist[eng].append(i)
            elif signal[i]:
                cnt[eng] += 1
                sig[i] = (sems[eng], cnt[eng])
        waited = {e: {} for e in self.ENGS}
        per_eng = {e: [] for e in self.ENGS}
        for i, o in enumerate(ops):
            per_eng[o[0]].append(i)

        def emit(eng_name, e):
            wd = waited[eng_name]
            for i in per_eng[eng_name]:
                _, fn, r, w, dma = ops[i]
                js = set(need[i])
                if prevdma[i] is not None:
                    js.add(prevdma[i])
                for j in sorted(js):
                    s, v = sig[j]
                    key = s.name if hasattr(s, "name") else id(s)
                    if wd.get(key, 0) >= v:
                        continue
                    e.wait_ge(s, v)
                    wd[key] = v
                inst = fn(e)
                if sig[i] is not None:
                    inst.then_inc(sig[i][0], 16 if dma else 1)

        self.final_waits = []
        with nc.Block() as block:
            @block.tensor
            def _(e):
                emit("pe", e)

            @block.scalar
            def _(e):
                emit("act", e)

            @block.vector
            def _(e):
                emit("dve", e)

            @block.gpsimd
            def _(e):
                emit("pool", e)

            @block.sync
            def _(e):
                emit("sp", e)
                for q in rings:
                    for k, s in enumerate(rings[q]):
                        tot = len([1 for x in range(dcnt[q]) if x % NDMA_RING == k])
                        if tot:
                            e.wait_ge(s, 16 * tot)
        self.es.close()
        return nc


D = 2048
KT = 16
IN_COLS = 12704
RMS_EPS = 1e-6


def emit_adaln(p, cvec, ada_w, ada_b, ncols, mod_bc, pss, wbuf, shared=None, wkey=None):
    if shared is None:
        shared = {}
    if "cbc" not in shared:
        ccol = p.sb([128, 2, KT], F32, "ccol")
        p.dma("sp", ccol[:], cvec.rearrange("v (kt q) -> q v kt", q=128), w=[ccol], allow_slow_non_contiguous=True)
        cact = p.sb([128, 2, KT], F32, "cact")
        p.op("act", lambda e: e.activation(out=cact[:], in_=ccol[:], func=AF.Silu), r=[ccol], w=[cact])
        cbc = p.sb([128, 2, KT, 128], BF16, "cbc")
        for v in range(2):
            p.op("dve", lambda e, v=v: e.tensor_copy(out=cbc[:, v], in_=cact[:, v, :, None].to_broadcast([128, KT, 128])),
                 r=[cact], w=[(cbc, v)])
        shared["cbc"] = cbc
    if "abbs" not in shared:
        shared["abbs"] = [p.sb([128, 512], F32, f"abb{i}") for i in range(2)]
    cbc, abbs = shared["cbc"], shared["abbs"]
    for g in range(ncols // 512):
        wb = wbuf[g % 2]
        for q in range(4):
            p.dma("pool", wb[:, 4 * q:4 * q + 4, :],
                  ada_w[:, g * 512:(g + 1) * 512].rearrange("(kt q) c -> q kt c", q=128)[:, 4 * q:4 * q + 4, :],
                  w=[(wb, q)])
        abb = abbs[g % 2]
        p.dma("sp", abb[:], ada_b[g * 512:(g + 1) * 512].partition_broadcast(128), w=[abb])
        for v in range(2):
            ps = pss[(2 * g + v) % len(pss)]
            for kt in range(KT):
                p.op("pe", lambda e, ps=ps, v=v, kt=kt, wb=wb: e.matmul(ps[:], lhsT=cbc[:, v, kt, :], rhs=wb[:, kt, :],
                                                                     start=(kt == 0), stop=(kt == KT - 1)),
                     r=[(cbc, v), (wb, kt // 4)], w=[ps])
            p.op("dve", lambda e, ps=ps, v=v, g=g, abb=abb: e.tensor_tensor(out=mod_bc[v][:, g * 512:(g + 1) * 512], in0=ps[:],
                                                                            in1=abb[:], op=ALU.add),
                 r=[ps, abb], w=[(mod_bc[v], g if wkey is None else wkey)])
    return shared


def emit_norm_mod_T(p, xin_tile_fn, ntiles, tile_rows, G1, SH, vsel, hT, ident, pst, tag):
    xb = [p.sb([128, D], F32, f"xb{tag}{i}") for i in range(2)]
    sq = p.sb([128, D], BF16, f"sq{tag}")
    hb = [p.sb([128, D], BF16, f"hb{tag}{i}") for i in range(2)]
    ss = [p.sb([128, 2], F32, f"ss{tag}{i}") for i in range(2)]
    for t in range(ntiles):
        rows = tile_rows(t)
        x_t, h_t, s_t = xb[t % 2], hb[t % 2], ss[t % 2]
        v = vsel(t)
        p.dma("sp", x_t[:rows, :], xin_tile_fn(t), w=[x_t])
        p.op("act", lambda e, x_t=x_t, s_t=s_t, rows=rows: e.activation(out=sq[:rows, :], in_=x_t[:rows, :], func=AF.Square,
                                                                        accum_out=s_t[:rows, 0:1]),
             r=[x_t], w=[sq, (s_t, 0)])
        p.op("dve", lambda e, s_t=s_t, rows=rows: e.tensor_scalar(out=s_t[:rows, 1:2], in0=s_t[:rows, 0:1], scalar1=1.0 / D,
                                                                  scalar2=RMS_EPS, op0=ALU.mult, op1=ALU.add),
             r=[(s_t, 0)], w=[(s_t, 1)])
        p.op("act", lambda e, s_t=s_t, rows=rows: e.sqrt(out=s_t[:rows, 1:2], in_=s_t[:rows, 1:2]),
             r=[(s_t, 1)], w=[(s_t, 1)])
        p.op("dve", lambda e, s_t=s_t, rows=rows: e.reciprocal(out=s_t[:rows, 1:2], in_=s_t[:rows, 1:2]),
             r=[(s_t, 1)], w=[(s_t, 1)])
        p.op("dve", lambda e, x_t=x_t, s_t=s_t, rows=rows, v=v: e.scalar_tensor_tensor(
            out=x_t[:rows, :], in0=x_t[:rows, :], scalar=s_t[:rows, 1:2], in1=G1[v][0][:rows, G1[v][1]:G1[v][1] + D], op0=ALU.mult, op1=ALU.mult),
            r=[x_t, (s_t, 1), G1[v][0]], w=[x_t])
        p.op("pool", lambda e, x_t=x_t, h_t=h_t, rows=rows, v=v: e.tensor_tensor(out=h_t[:rows, :], in0=x_t[:rows, :],
                                                                                 in1=SH[v][0][:rows, SH[v][1]:SH[v][1] + D], op=ALU.add),
             r=[x_t, SH[v][0]], w=[h_t])
        for q in range(4):
            ps = pst[(4 * t + q) % len(pst)]
            for j in range(4):
                kt = 4 * q + j
                p.op("pe", lambda e, ps=ps, h_t=h_t, kt=kt, j=j, rows=rows: e.transpose(
                    out=ps[:, j * 128:j * 128 + rows], in_=h_t[:rows, kt * 128:(kt + 1) * 128], identity=ident[:rows, :rows]),
                    r=[h_t, ident], w=[ps])
            eng = "act" if q % 2 == 0 else "dve"
            if eng == "act":
                p.op("act", lambda e, ps=ps, q=q, t=t, rows=rows: e.copy(
                    out=hT[:, 4 * q:4 * q + 4, t * 128:t * 128 + rows],
                    in_=ps[:].rearrange("p (j c) -> p j c", j=4)[:, :, :rows]),
                    r=[ps], w=[(hT, t)])
            else:
                p.op("dve", lambda e, ps=ps, q=q, t=t, rows=rows: e.tensor_copy(
                    out=hT[:, 4 * q:4 * q + 4, t * 128:t * 128 + rows],
                    in_=ps[:].rearrange("p (j c) -> p j c", j=4)[:, :, :rows]),
                    r=[ps], w=[(hT, t)])


def make_ident(p, dtype=BF16, name="ident"):
    return None


def build_l1(n_lat_c=2048, n_ctx_c=64):
    NTOK = n_lat_c + n_ctx_c
    p = Prog()
    xs = p.dram("xs", [NTOK, D])
    cvec = p.dram("cvec", [2, D])
    ada_w = p.dram("ada_w", [D, 4096])
    ada_b = p.dram("ada_b", [4096])
    ng = p.dram("norm_g", [D])
    w_in = p.dram("w_in", [D, IN_COLS])
    identd = p.dram("identd", [128, 128])
    PT = p.dram("PT", [IN_COLS, NTOK], kind="ExternalOutput")

    pss = [p.ps([128, 512], F32, f"pacc{i}") for i in range(6)]
    pst = [p.ps([128, 512], BF16, f"ptr{i}") for i in range(2)]
    identf = p.sb([128, 128], F32, "identf")
    ident = p.sb([128, 128], BF16, "ident")
    p.dma("sp", identf[:], identd, w=[identf])
    p.op("dve", lambda e: e.tensor_copy(out=ident[:], in_=identf[:]), r=[identf], w=[ident])

    mod = [p.sb([128, 4096], F32, f"mod{v}") for v in range(2)]
    wbuf = [p.sb([128, KT, 512], BF16, f"win{i}") for i in range(2)]
    emit_adaln(p, cvec, ada_w, ada_b, 4096, mod, pss, wbuf)
    gbc = p.sb([128, D], F32, "gbc")
    p.dma("sp", gbc[:], ng.partition_broadcast(128), w=[gbc])
    for v in range(2):
        p.op("dve", lambda e, v=v: e.scalar_tensor_tensor(out=mod[v][:, D:2 * D], in0=mod[v][:, D:2 * D], scalar=1.0, in1=gbc[:],
                                                           op0=ALU.add, op1=ALU.mult), r=[mod[v], gbc], w=[mod[v]])
    G1 = [(mod[v], D) for v in range(2)]
    SH = [(mod[v], 0) for v in range(2)]

    hT = p.sb([128, KT, NTOK], BF16, "hT")
    tls = [(t, min(128, NTOK - t)) for t in range(0, NTOK, 128)]
    ntl = len(tls)
    emit_norm_mod_T(p, lambda t: xs[tls[t][0]:tls[t][0] + tls[t][1], :], ntl,
                    lambda t: tls[t][1], G1, SH, lambda t: 0 if tls[t][0] < n_lat_c else 1, hT, ident, pst, "a")

    ost = [p.sb([128, NTOK], F32, f"ost{i}") for i in range(2)]
    ngrp = (IN_COLS + 511) // 512
    tgs = [(t, min(512, NTOK - t)) for t in range(0, NTOK, 512)]
    cti = 0
    for g in range(ngrp):
        c0 = g * 512
        gc = min(512, IN_COLS - c0)
        wb = wbuf[g % 2]
        for q in range(4):
            p.dma("pool", wb[:, 4 * q:4 * q + 4, :gc],
                  w_in[:, c0:c0 + gc].rearrange("(kt q) c -> q kt c", q=128)[:, 4 * q:4 * q + 4, :],
                  w=[(wb, q)])
        for ct in range((gc + 127) // 128):
            cw = min(128, gc - ct * 128)
            o_t = ost[cti % 2]
            for gi, (t0, tn) in enumerate(tgs):
                ps = pss[(cti * len(tgs) + gi) % len(pss)]
                for kt in range(KT):
                    p.op("pe", lambda e, ps=ps, wb=wb, kt=kt, ct=ct, cw=cw, t0=t0, tn=tn: e.matmul(
                        ps[:cw, :tn], lhsT=wb[:, kt, ct * 128:ct * 128 + cw], rhs=hT[:, kt, t0:t0 + tn],
                        start=(kt == 0), stop=(kt == KT - 1)),
                        r=[(wb, kt // 4)] + [(hT, t0 // 128 + j) for j in range((tn + 127) // 128)], w=[ps])
                if gi % 2 == 0:
                    p.op("act", lambda e, ps=ps, o_t=o_t, cw=cw, t0=t0, tn=tn: e.copy(out=o_t[:cw, t0:t0 + tn], in_=ps[:cw, :tn]),
                         r=[ps], w=[(o_t, gi)])
                else:
                    p.op("dve", lambda e, ps=ps, o_t=o_t, cw=cw, t0=t0, tn=tn: e.tensor_copy(out=o_t[:cw, t0:t0 + tn], in_=ps[:cw, :tn]),
                         r=[ps], w=[(o_t, gi)])
            p.dma("sp", PT[c0 + ct * 128:c0 + ct * 128 + cw, :], o_t[:cw, :], r=[o_t])
            cti += 1
    return p.build()


C = 32
TB = 128
NCH = TB // C
CDEC = 0.6065306597126334
NH = 4

def pc_layout():
    idx = {}
    n = 0
    for h in range(NH):
        for a in range(3):
            idx[("mu", h, a)] = n; n += 1
    for i in range(4):
        idx[("mulr", i)] = n; n += 1
    for d in range(2):
        for h in range(NH):
            idx[("w0", d, h)] = n; n += 1
            idx[("a0", d, h)] = n; n += 1
    for h in range(NH):
        idx[("kk", h)] = n; n += 1
        idx[("ka", h)] = n; n += 1
        idx[("rk", h)] = n; n += 1
    return idx, n


PCI, NPC = pc_layout()


def build_l2r(n_ctx, n_lat):
    Tt = n_ctx + n_lat
    NB = Tt // TB
    NBC = n_ctx // TB
    p = Prog()
    RB = p.dram("RB", [NH, 3, 64, Tt])
    RBs = p.dram("RBs", [NH, 3, 64, Tt])
    LR = p.dram("LR", [4, 64, Tt])
    LRs = p.dram("LRs", [4, 64, Tt])
    GD = p.dram("GD", [160, Tt])
    GDs = p.dram("GDs", [160, Tt])
    PCd = p.dram("PC", [64, NPC])
    MUG = p.dram("MUG", [128, 2])
    WUP = p.dram("WUP", [2, 64, NH * 64])
    AUP = p.dram("AUP", [2, 64, NH * 64])
    GUP = p.dram("GUP", [160, NH * 64])
    CST = p.dram("CST", [64, 64 + 64 + 128 * 2 + 32 * 2 + TB])
    OUT = p.dram("OUT", [2, NH, 64, Tt], kind="ExternalOutput")
    BON = p.dram("BON", [NH, 64, Tt], kind="ExternalOutput")
    GO = p.dram("GO", [NH, 64, Tt], kind="ExternalOutput")

    B = [p.ps([128, 512], F32, f"bank{i}") for i in range(8)]
    pcs = p.sb([64, NPC], F32, "pcs")
    p.dma("sp", pcs[:], PCd, w=[pcs])
    mug = p.sb([128, 2], F32, "mug")
    p.dma("sp", mug[:], MUG, w=[mug])
    wup = p.sb([64, 2, NH * 64], F32, "wup")
    aup = p.sb([64, 2, NH * 64], F32, "aup")
    p.dma("sp", wup[:], WUP.rearrange("d r c -> r d c"), w=[wup])
    p.dma("sp", aup[:], AUP.rearrange("d r c -> r d c"), w=[aup])
    gup = p.sb([128, 2, NH * 64], F32, "gup")
    p.dma("sp", gup[:, 0, :], GUP[0:128, :], w=[(gup, 0)])
    p.dma("sp", gup[0:32, 1, :], GUP[128:160, :], w=[(gup, 1)])
    cst = p.sb([64, 64 + 64 + 256 + 64 + TB], F32, "cst")
    p.dma("sp", cst[:], CST, w=[cst])
    ident = cst[:, 0:64]
    ones = cst[:, 64:128]

    def mask_s(d):
        return cst[0:32, 128 + 128 * d:128 + 128 * (d + 1)]

    def mask_l(d):
        return cst[0:32, 384 + 32 * d:384 + 32 * (d + 1)]
    rmask = cst[:, 448:448 + TB]

    def col(key):
        i = PCI[key]
        return pcs[:, i:i + 1]

    state = {}
    for d in range(2):
        for h in range(NH):
            st = [p.sb([64, 64], F32, f"A{d}{h}{i}") for i in range(2)]
            p.op("dve", lambda e, t=st[0]: e.memset(t[:], 0.0), w=[st[0]])
            state[(d, h)] = [st, 0]

    def mk(shape, name, n=2, dt=F32):
        return [p.sb(shape, dt, f"{name}{i}") for i in range(n)]
    lrb = mk([64, 4, TB], "lrb"); lrs = mk([64, 4, TB], "lrs")
    twd = mk([64, 2, TB], "twd")
    gdb = mk([128, 2, TB], "gdb", 1); gds = mk([128, 2, TB], "gds", 1)
    rkv = mk([64, 3, TB], "rkv"); rks = mk([64, 3, TB], "rks")
    sig = mk([64, TB], "sig"); css = mk([64, TB], "css")
    aa = mk([64, 2, TB], "aa")
    tmp = mk([64, 6, TB], "tmp")
    kap = mk([64, TB], "kap")
    kd = mk([64, 2, TB], "kd")
    eee = mk([64, 3, TB], "eee")
    pcc = mk([64, NCH], "pcc")
    AR = mk([64, NCH, 2, C], "AR"); BK = mk([64, NCH, 2, C], "BK")
    SM = mk([32, NCH, 128], "SM"); LM = mk([32, NCH, 32], "LM")
    PP = mk([32, NCH, 64], "PP", 2)
    TT = mk([32, NCH, 32], "TT")
    TOK = mk([32, NCH, 192], "TOK")
    Ysb = mk([32, 64], "Ysb"); Usb = mk([32, 64], "Usb")
    ost = mk([64, TB], "ost"); bst = mk([64, TB], "bst"); gst = mk([64, TB], "gst")

    cnt = [0]
    ccnt = [0]

    def visit_common(d, blk):
        k = ccnt[0] % 2
        ccnt[0] += 1
        t0 = blk * TB
        lb, ls, tw = lrb[k], lrs[k], twd[k]
        p.dma("pool", lb[:], LR[:, :, t0:t0 + TB].rearrange("a q t -> q a t"), w=[lb])
        p.dma("pool", ls[:], LRs[:, :, t0:t0 + TB].rearrange("a q t -> q a t"), w=[ls])
        p.op("dve", lambda e: e.tensor_tensor(out=ls[:], in0=ls[:], in1=lb[:], op=ALU.subtract), r=[ls, lb], w=[ls])
        for i in range(4):
            p.op("dve", lambda e, i=i: e.scalar_tensor_tensor(out=lb[:, i, :], in0=ls[:, i, :], scalar=col(("mulr", i)),
                                                               in1=lb[:, i, :], op0=ALU.mult, op1=ALU.add),
                 r=[ls, lb, pcs], w=[lb])
        p.op("act", lambda e: e.activation(out=tw[:], in_=lb[:, 0:2, :], func=AF.Tanh), r=[lb], w=[tw])
        return lb, tw

    def visit_g(blk):
        t0 = blk * TB
        gb, gs = gdb[0], gds[0]
        p.dma("pool", gb[:, 0, :], GD[0:128, t0:t0 + TB], w=[gb])
        p.dma("pool", gb[0:32, 1, :], GD[128:160, t0:t0 + TB], w=[gb])
        p.dma("pool", gs[:, 0, :], GDs[0:128, t0:t0 + TB], w=[gs])
        p.dma("pool", gs[0:32, 1, :], GDs[128:160, t0:t0 + TB], w=[gs])
        for j, rows in ((0, 128), (1, 32)):
            p.op("dve", lambda e, j=j, rows=rows: e.tensor_tensor(out=gs[:rows, j, :], in0=gs[:rows, j, :], in1=gb[:rows, j, :],
                                                                   op=ALU.subtract), r=[gs, gb], w=[gs])
            p.op("dve", lambda e, j=j, rows=rows: e.scalar_tensor_tensor(out=gb[:rows, j, :], in0=gs[:rows, j, :],
                                                                          scalar=mug[:rows, j:j + 1], in1=gb[:rows, j, :],
                                                                          op0=ALU.mult, op1=ALU.add), r=[gs, gb, mug], w=[gb])
            p.op("act", lambda e, j=j, rows=rows: e.activation(out=gb[:rows, j, :], in_=gb[:rows, j, :], func=AF.Sigmoid),
                 r=[gb], w=[gb])
        for h in range(NH):
            g_t = gst[h % 2]
            ps = B[0]
            p.op("pe", lambda e, h=h: e.matmul(ps[0:64, 384:512], lhsT=gup[:, 0, h * 64:(h + 1) * 64], rhs=gb[:, 0, :],
                                               start=True, stop=False), r=[gup, gb], w=[ps])
            p.op("pe", lambda e, h=h: e.matmul(ps[0:64, 384:512], lhsT=gup[0:32, 1, h * 64:(h + 1) * 64], rhs=gb[0:32, 1, :],
                                               start=False, stop=True), r=[gup, gb], w=[ps])
            p.op("act", lambda e, g_t=g_t: e.copy(out=g_t[:], in_=ps[0:64, 384:512]), r=[ps], w=[g_t])
            p.dma("sp", GO[h, :, t0:t0 + TB], g_t[:], r=[g_t])

    def visit(d, blk, h, lb, tw):
        k = cnt[0] % 2
        cnt[0] += 1
        t0 = blk * TB
        x, xs_, sg, cs, a_t, tp, kp, kd_t, ee, pc_t = rkv[k], rks[k], sig[k], css[k], aa[k], tmp[k], kap[k], kd[k], eee[k], pcc[k]
        ar, bk, sm, lm, tt, tok = AR[k], BK[k], SM[k], LM[k], TT[k], TOK[k]
        o_t = ost[k]
        hs = slice(h * 64, (h + 1) * 64)
        p.dma("pool", x[:], RB[h, :, :, t0:t0 + TB].rearrange("a q t -> q a t"), w=[x])
        p.dma("pool", xs_[:], RBs[h, :, :, t0:t0 + TB].rearrange("a q t -> q a t"), w=[xs_])
        p.op("dve", lambda e: e.tensor_tensor(out=xs_[:], in0=xs_[:], in1=x[:], op=ALU.subtract), r=[xs_, x], w=[xs_])
        for a in range(3):
            p.op("dve", lambda e, a=a: e.scalar_tensor_tensor(out=x[:, a, :], in0=xs_[:, a, :], scalar=col(("mu", h, a)),
                                                               in1=x[:, a, :], op0=ALU.mult, op1=ALU.add),
                 r=[xs_, x, pcs], w=[x])
        mr, mkk, mv = x[:, 0, :], x[:, 1, :], x[:, 2, :]
        p.op("pe", lambda e: e.matmul(B[0][0:64, 0:128], lhsT=wup[:, d, hs], rhs=tw[:, d, :], start=True, stop=True),
             r=[wup, tw], w=[B[0]])
        p.op("act", lambda e: e.activation(out=sg[:], in_=B[0][0:64, 0:128], func=AF.Sigmoid, bias=col(("w0", d, h))),
             r=[B[0], pcs], w=[sg])
        dirs = (0, 1) if d == 0 else (1,)
        for dd in dirs:
            p.op("pe", lambda e, dd=dd: e.matmul(B[0][0:64, 128:256], lhsT=aup[:, dd, hs], rhs=lb[:, 2 + dd, :], start=True, stop=True),
                 r=[aup, lb], w=[B[0]])
            p.op("act", lambda e, dd=dd: e.activation(out=a_t[:, dd, :], in_=B[0][0:64, 128:256], func=AF.Sigmoid,
                                                       bias=col(("a0", dd, h))), r=[B[0], pcs], w=[a_t])
        kkt = tp[:, 0, :]
        p.op("dve", lambda e: e.tensor_scalar(out=kkt, in0=mkk, scalar1=col(("kk", h)), scalar2=None, op0=ALU.mult),
             r=[x, pcs], w=[(tp, 0)])
        p.op("act", lambda e: e.activation(out=tp[:, 1, :], in_=kkt, func=AF.Square), r=[(tp, 0)], w=[(tp, 1)])
        p.op("pe", lambda e: e.matmul(B[0][0:64, 256:384], lhsT=ones, rhs=tp[:, 1, :], start=True, stop=True),
             r=[cst, (tp, 1)], w=[B[0]])
        p.op("act", lambda e: e.sqrt(out=tp[:, 1, :], in_=B[0][0:64, 256:384]), r=[B[0]], w=[(tp, 1)])
        p.op("dve", lambda e: e.tensor_scalar(out=tp[:, 1, :], in0=tp[:, 1, :], scalar1=1e-12, scalar2=None, op0=ALU.max),
             r=[(tp, 1)], w=[(tp, 1)])
        p.op("dve", lambda e: e.reciprocal(out=tp[:, 1, :], in_=tp[:, 1, :]), r=[(tp, 1)], w=[(tp, 1)])
        p.op("dve", lambda e: e.tensor_tensor(out=kp[:], in0=kkt, in1=tp[:, 1, :], op=ALU.mult), r=[(tp, 0), (tp, 1)], w=[kp])
        for dd in dirs:
            p.op("dve", lambda e, dd=dd: e.tensor_scalar(out=tp[:, 2, :], in0=a_t[:, dd, :], scalar1=-1.0, scalar2=col(("ka", h)),
                                                          op0=ALU.add, op1=ALU.mult), r=[a_t, pcs], w=[(tp, 2)])
            p.op("dve", lambda e, dd=dd: e.scalar_tensor_tensor(out=kd_t[:, dd, :], in0=tp[:, 2, :], scalar=1.0, in1=mkk,
                                                                 op0=ALU.add, op1=ALU.mult), r=[(tp, 2), x], w=[(kd_t, dd)])
        if d == 0:
            b_t = bst[k]
            p.op("dve", lambda e: e.tensor_tensor(out=tp[:, 2, :], in0=kd_t[:, 0, :], in1=kd_t[:, 1, :], op=ALU.add),
                 r=[kd_t], w=[(tp, 2)])
            p.op("dve", lambda e: e.scalar_tensor_tensor(out=tp[:, 2, :], in0=mr, scalar=col(("rk", h)), in1=tp[:, 2, :],
                                                          op0=ALU.mult, op1=ALU.mult), r=[x, pcs, (tp, 2)], w=[(tp, 2)])
            p.op("pe", lambda e: e.matmul(B[0][0:64, 384:512], lhsT=ones, rhs=tp[:, 2, :], start=True, stop=True),
                 r=[cst, (tp, 2)], w=[B[0]])
            p.op("dve", lambda e: e.tensor_tensor(out=b_t[:], in0=B[0][0:64, 384:512], in1=mv, op=ALU.mult),
                 r=[B[0], x], w=[b_t])
            p.dma("sp", BON[h, :, t0:t0 + TB], b_t[:], r=[b_t])
        p.op("dve", lambda e: e.tensor_tensor_scan(out=cs[:], data0=rmask, data1=sg[:], initial=0.0, op0=ALU.mult, op1=ALU.add),
             r=[cst, sg], w=[cs])
        cs3 = cs[:].rearrange("q (c t) -> q c t", t=C)
        tot = cs3[:, :, C - 1:C]
        incl, excl = tp[:, 3, :], tp[:, 4, :]
        if d == 0:
            p.op("dve", lambda e: e.tensor_tensor(out=excl, in0=cs[:], in1=sg[:], op=ALU.subtract), r=[cs, sg], w=[(tp, 4)])
            incl_ap = cs[:]
            incl_res = cs
        else:
            p.op("dve", lambda e: e.tensor_tensor(out=excl.rearrange("q (c t) -> q c t", t=C), in0=tot.to_broadcast([64, NCH, C]),
                                                  in1=cs3, op=ALU.subtract), r=[cs], w=[(tp, 4)])
            p.op("dve", lambda e: e.tensor_tensor(out=incl, in0=excl, in1=sg[:], op=ALU.add), r=[(tp, 4), sg], w=[(tp, 3)])
            incl_ap = incl
            incl_res = (tp, 3)
        p.op("act", lambda e: e.activation(out=ee[:, 0, :], in_=incl_ap, func=AF.Exp, scale=-CDEC), r=[incl_res], w=[(ee, 0)])
        p.op("act", lambda e: e.activation(out=ee[:, 1, :], in_=excl, func=AF.Exp, scale=-CDEC), r=[(tp, 4)], w=[(ee, 1)])
        p.op("act", lambda e: e.activation(out=ee[:, 2, :], in_=incl_ap, func=AF.Exp, scale=CDEC), r=[incl_res], w=[(ee, 2)])
        p.op("act", lambda e: e.activation(out=pc_t[:], in_=tot.rearrange("q c o -> q (c o)"), func=AF.Exp, scale=-CDEC),
             r=[cs], w=[pc_t])

        def v3(ap):
            return ap.rearrange("q (c t) -> q c t", t=C)
        p.op("dve", lambda e: e.scalar_tensor_tensor(out=ar[:, :, 0, :], in0=v3(kp[:]), scalar=-1.0, in1=v3(ee[:, 1, :]),
                                                      op0=ALU.mult, op1=ALU.mult), r=[kp, (ee, 1)], w=[(ar, 0)])
        p.op("dve", lambda e: e.tensor_tensor(out=ar[:, :, 1, :], in0=v3(mr), in1=v3(ee[:, 0, :]), op=ALU.mult),
             r=[x, (ee, 0)], w=[(ar, 1)])
        p.op("dve", lambda e: e.tensor_tensor(out=tp[:, 5, :], in0=kp[:], in1=a_t[:, d, :], op=ALU.mult), r=[kp, a_t], w=[(tp, 5)])
        p.op("dve", lambda e: e.tensor_tensor(out=bk[:, :, 0, :], in0=v3(tp[:, 5, :]), in1=v3(ee[:, 2, :]), op=ALU.mult),
             r=[(tp, 5), (ee, 2)], w=[(bk, 0)])
        p.op("dve", lambda e: e.tensor_tensor(out=bk[:, :, 1, :], in0=v3(kd_t[:, d, :]), in1=v3(ee[:, 2, :]), op=ALU.mult),
             r=[(kd_t, d), (ee, 2)], w=[(bk, 1)])
        ps_s = B[1][0:32, :].rearrange("q (c x) -> q c x", x=128)
        ps_l = B[2][0:32, 0:128].rearrange("q (c x) -> q c x", x=32)
        ps_t = B[2][0:32, 128:256].rearrange("q (c x) -> q c x", x=32)
        ps_d = B[3][0:32, 0:256].rearrange("q (c x) -> q c x", x=64)
        for c in range(NCH):
            arc = ar[:, c, :, :].rearrange("q a t -> q (a t)")
            p.op("pe", lambda e, c=c, arc=arc: e.matmul(ps_s[:, c, 0:64], lhsT=bk[:, c, 0, :], rhs=arc, start=True, stop=True),
                 r=[bk, ar], w=[B[1]])
            p.op("pe", lambda e, c=c, arc=arc: e.matmul(ps_s[:, c, 64:128], lhsT=bk[:, c, 1, :], rhs=arc, start=True, stop=True),
                 r=[bk, ar], w=[B[1]])
            p.op("pe", lambda e, c=c: e.matmul(ps_l[:, c, :], lhsT=ar[:, c, 0, :], rhs=bk[:, c, 0, :], start=True, stop=True),
                 r=[bk, ar], w=[B[2]])
        p.op("dve", lambda e: e.tensor_tensor(out=sm[:], in0=ps_s, in1=mask_s(d)[:, None, :].to_broadcast([32, NCH, 128]), op=ALU.mult),
             r=[B[1], cst], w=[sm])
        p.op("dve", lambda e: e.tensor_tensor(out=lm[:], in0=ps_l, in1=mask_l(d)[:, None, :].to_broadcast([32, NCH, 32]), op=ALU.mult),
             r=[B[2], cst], w=[lm])
        p.op("dve", lambda e: e.tensor_tensor(out=tt[:], in0=sm[:, :, 0:32], in1=ident[0:32, None, 0:32].to_broadcast([32, NCH, 32]),
                                               op=ALU.add), r=[sm, cst], w=[tt])
        Pcur = lambda c: lm[:, c, :]
        Ptcur = lambda c: sm[:, c, 0:32]
        Pres, Ptres = lm, sm
        for lvl in range(4):
            pp = PP[lvl % 2]
            last = (lvl == 3)
            for c in range(NCH):
                p.op("pe", lambda e, c=c, Pc=Pcur, Ptc=Ptcur: e.matmul(ps_d[:, c, 0:32], lhsT=Ptc(c), rhs=Pc(c), start=True, stop=True),
                     r=[Pres, Ptres], w=[B[3]])
                if not last:
                    p.op("pe", lambda e, c=c, Pc=Pcur, Ptc=Ptcur: e.matmul(ps_d[:, c, 32:64], lhsT=Pc(c), rhs=Ptc(c), start=True, stop=True),
                         r=[Pres, Ptres], w=[B[3]])
            if last:
                p.op("act", lambda e, pp=pp: e.copy(out=pp[:, :, 0:32], in_=ps_d[:, :, 0:32]), r=[B[3]], w=[pp])
            else:
                p.op("act", lambda e, pp=pp: e.copy(out=pp[:], in_=ps_d), r=[B[3]], w=[pp])
            for c in range(NCH):
                p.op("pe", lambda e, c=c, pp=pp: e.matmul(ps_t[:, c, :], lhsT=pp[:, c, 0:32], rhs=tt[:, c, :], start=True, stop=True),
                     r=[pp, tt], w=[B[2]])
            p.op("dve", lambda e: e.tensor_tensor(out=tt[:], in0=tt[:], in1=ps_t, op=ALU.add), r=[tt, B[2]], w=[tt])
            Pcur = lambda c, pp=pp: pp[:, c, 0:32]
            Ptcur = lambda c, pp=pp: pp[:, c, 32:64]
            Pres, Ptres = pp, pp
        for half in range(2):
            bnk = B[4]
            psx = bnk[0:32, :].rearrange("q (c x) -> q c x", x=256)
            for cc in range(2):
                c = 2 * half + cc
                p.op("pe", lambda e, c=c, cc=cc, psx=psx: e.transpose(out=psx[:, cc, 0:64], in_=bk[:, c, 0, :], identity=ident),
                     r=[bk, cst], w=[bnk])
                p.op("pe", lambda e, c=c, cc=cc, psx=psx: e.transpose(out=psx[:, cc, 64:128], in_=bk[:, c, 1, :], identity=ident),
                     r=[bk, cst], w=[bnk])
                p.op("pe", lambda e, c=c, cc=cc, psx=psx: e.transpose(out=psx[:, cc, 128:192], in_=mv[:, c * C:(c + 1) * C], identity=ident),
                     r=[x, cst], w=[bnk])
            p.op("act", lambda e, half=half, psx=psx: e.copy(out=tok[:, 2 * half:2 * half + 2, :], in_=psx[:, :, 0:192]),
                 r=[bnk], w=[(tok, half)])
        st, cur = state[(d, h)]
        order = range(NCH) if d == 0 else range(NCH - 1, -1, -1)
        for c in order:
            A0 = st[cur]
            A1 = st[1 - cur]
            y_t, u_t = Ysb[k], Usb[k]
            psY = B[5][0:32, 0:64]
            psU = B[5][0:32, 64:128]
            psO = B[6][0:64, 0:32]
            psA = B[7][0:64, 64:128]
            p.op("pe", lambda e, c=c, A0=A0: e.matmul(psY, lhsT=ar[:, c, 0, :], rhs=A0[:], start=True, stop=False),
                 r=[ar, A0], w=[B[5]])
            p.op("pe", lambda e, c=c: e.matmul(psY, lhsT=sm[:, c, 64:96], rhs=tok[:, c, 128:192], start=False, stop=True),
                 r=[sm, (tok, c // 2)], w=[B[5]])
            p.op("act", lambda e: e.copy(out=y_t[:], in_=psY), r=[B[5]], w=[y_t])
            p.op("pe", lambda e, c=c: e.matmul(psU, lhsT=tt[:, c, :], rhs=y_t[:], start=True, stop=True),
                 r=[tt, y_t], w=[B[5]])
            p.op("dve", lambda e: e.tensor_copy(out=u_t[:], in_=psU), r=[B[5]], w=[u_t])
            p.op("pe", lambda e, c=c, A0=A0: e.matmul(psO, lhsT=A0[:], rhs=ar[:, c, 1, :], start=True, stop=False),
                 r=[ar, A0], w=[B[6]])
            p.op("pe", lambda e, c=c: e.matmul(psO, lhsT=u_t[:], rhs=sm[:, c, 32:64], start=False, stop=False),
                 r=[u_t, sm], w=[B[6]])
            p.op("pe", lambda e, c=c: e.matmul(psO, lhsT=tok[:, c, 128:192], rhs=sm[:, c, 96:128], start=False, stop=True),
                 r=[(tok, c // 2), sm], w=[B[6]])
            p.op("act", lambda e, c=c: e.copy(out=o_t[:, c * C:(c + 1) * C], in_=psO), r=[B[6]], w=[(o_t, c)])
            p.op("pe", lambda e, A0=A0: e.matmul(psA, lhsT=ident, rhs=A0[:], start=True, stop=False),
                 r=[cst, A0], w=[B[7]])
            p.op("pe", lambda e, c=c: e.matmul(psA, lhsT=tok[:, c, 0:64], rhs=u_t[:], start=False, stop=False),
                 r=[(tok, c // 2), u_t], w=[B[7]])
            p.op("pe", lambda e, c=c: e.matmul(psA, lhsT=tok[:, c, 64:128], rhs=tok[:, c, 128:192], start=False, stop=True),
                 r=[(tok, c // 2)], w=[B[7]])
            p.op("dve", lambda e, c=c, A1=A1: e.tensor_scalar(out=A1[:], in0=psA, scalar1=pc_t[:, c:c + 1], scalar2=None, op0=ALU.mult),
                 r=[B[7], pc_t], w=[A1])
            cur = 1 - cur
        state[(d, h)][1] = cur
        p.dma("sp", OUT[d, h, :, t0:t0 + TB], o_t[:], r=[o_t])

    fwd_blocks = list(range(NB))
    bwd_blocks = list(range(NBC - 1, -1, -1)) + list(range(NB - 1, NBC - 1, -1))
    for s in range(NB):
        for d, blk in ((0, fwd_blocks[s]), (1, bwd_blocks[s])):
            lb, tw = visit_common(d, blk)
            if d == 0:
                visit_g(blk)
            for h in range(NH):
                visit(d, blk, h, lb, tw)
    return p.build()


def l2r_consts():
    cst = np.zeros((64, 64 + 64 + 256 + 64 + TB), np.float32)
    cst[:, 0:64] = np.eye(64)
    cst[:, 64:128] = 1.0
    r = np.arange(32)[:, None]
    cc = np.arange(32)[None, :]
    for d in range(2):
        strict = (r < cc) if d == 0 else (r > cc)
        incl = (r <= cc) if d == 0 else (r >= cc)
        cst[0:32, 128 + 128 * d:128 + 128 * (d + 1)] = np.concatenate([strict, incl, strict, incl], 1)
        cst[0:32, 384 + 32 * d:384 + 32 * (d + 1)] = (cc < r) if d == 0 else (cc > r)
    rm = np.ones(TB, np.float32)
    rm[::C] = 0.0
    cst[:, 448:448 + TB] = rm[None, :]
    return cst


HC = 32
HTB = 128
HNCH = HTB // HC
HNH = 2
ECLAMP = 60.0


def build_l2h(n_ctx, n_lat, layer):
    C, TB, NCH, NH = HC, HTB, HNCH, HNH
    Tt = n_ctx + n_lat
    NB = Tt // TB
    NBC = n_ctx // TB
    p = Prog()
    HB = p.dram("HB", [NH, 4, 128, Tt])
    LBL = p.dram("LBL", [128, 2 * 2 * NH])
    CST = p.dram("HCST", [128, 128 + 64 + TB])
    OUT = p.dram("HOUT", [2, NH, 128, Tt], kind="ExternalOutput")

    B = [p.ps([128, 512], F32, f"bank{i}") for i in range(8)]
    cst = p.sb([128, 128 + 64 + TB], F32, "hcst")
    p.dma("sp", cst[:], CST, w=[cst])
    ident = cst[:, 0:128]
    rmask = cst[:, 192:192 + TB]

    def mask(d):
        return cst[0:32, 128 + 32 * d:128 + 32 * (d + 1)]

    lbl = p.sb([128, 2, 2 * NH], F32, "lbl")
    p.dma("sp", lbl[:], LBL.rearrange("q (l x) -> q l x", l=2), w=[lbl])
    lb = p.sb([128, 2 * NH], F32, "lb")
    oml = p.sb([128, 2 * NH], F32, "oml")
    p.op("dve", lambda e: e.tensor_tensor(out=lb[:], in0=lbl[:, 1, :], in1=lbl[:, 0, :], op=ALU.subtract), r=[lbl], w=[lb])
    p.op("act", lambda e: e.activation(out=lb[:], in_=lb[:], func=AF.Sigmoid), r=[lb], w=[lb])
    p.op("dve", lambda e: e.tensor_scalar(out=lb[:], in0=lb[:], scalar1=float(layer), scalar2=None, op0=ALU.mult), r=[lb], w=[lb])
    p.op("dve", lambda e: e.tensor_scalar(out=oml[:], in0=lb[:], scalar1=-1.0, scalar2=1.0, op0=ALU.mult, op1=ALU.add), r=[lb], w=[oml])

    state = {}
    for d in range(2):
        for h in range(NH):
            st = [p.sb([128, 128], F32, f"S{d}{h}{i}") for i in range(2)]
            p.op("dve", lambda e, t=st[0]: e.memset(t[:], 0.0), w=[st[0]])
            state[(d, h)] = [st, 0]

    def mk(shape, name, n=2, dt=F32):
        return [p.sb(shape, dt, f"{name}{i}") for i in range(n)]
    X = mk([128, 3, TB], "hx")
    W = mk([128, 8, TB], "hw")
    QB = mk([128, TB], "hqb"); KB = mk([128, TB], "hkb"); KE = mk([128, TB], "hke")
    EBT = mk([128, NCH], "hebt")
    SCM = mk([32, NCH, 32], "hscm")
    TOK = mk([32, NCH, 256], "htok")
    OST = mk([128, TB], "host")
    cnt = [0]

    def visit(d, blk, h):
        k = cnt[0] % 2
        cnt[0] += 1
        t0 = blk * TB
        x, w, qb, kb, ke, ebt, scm, tok, o_t = X[k], W[k], QB[k], KB[k], KE[k], EBT[k], SCM[k], TOK[k], OST[k]
        ci = d * NH + h
        lbc, omc = lb[:, ci:ci + 1], oml[:, ci:ci + 1]
        p.dma("pool", x[:, 0, :], HB[h, 0, :, t0:t0 + TB], w=[(x, 0)])
        p.dma("pool", x[:, 1, :], HB[h, 1 + d, :, t0:t0 + TB], w=[(x, 1)])
        p.dma("pool", x[:, 2, :], HB[h, 3, :, t0:t0 + TB], w=[(x, 2)])
        qa, s, sn, f, lf, sc, binc, tmp = (w[:, i, :] for i in range(8))
        p.op("act", lambda e: e.activation(out=qa, in_=x[:, 0, :], func=AF.Silu), r=[(x, 0)], w=[(w, 0)])
        p.op("act", lambda e: e.activation(out=s, in_=x[:, 1, :], func=AF.Sigmoid), r=[(x, 1)], w=[(w, 1)])
        p.op("act", lambda e: e.activation(out=sn, in_=x[:, 1, :], func=AF.Sigmoid, scale=-1.0), r=[(x, 1)], w=[(w, 2)])
        p.op("dve", lambda e: e.tensor_scalar(out=f, in0=s, scalar1=omc, scalar2=lbc, op0=ALU.mult, op1=ALU.add),
             r=[(w, 1), oml, lb], w=[(w, 3)])
        p.op("act", lambda e: e.activation(out=lf, in_=f, func=AF.Ln), r=[(w, 3)], w=[(w, 4)])
        p.op("dve", lambda e: e.tensor_scalar(out=sn, in0=sn, scalar1=omc, scalar2=None, op0=ALU.mult), r=[(w, 2), oml], w=[(w, 2)])
        kk = sn
        p.op("dve", lambda e: e.tensor_tensor_scan(out=sc, data0=rmask, data1=lf, initial=0.0, op0=ALU.mult, op1=ALU.add),
             r=[cst, (w, 4)], w=[(w, 5)])
        sc3 = sc.rearrange("q (c t) -> q c t", t=C)
        tot = sc3[:, :, C - 1:C]

        def v3(ap):
            return ap.rearrange("q (c t) -> q c t", t=C)
        if d == 0:
            binc_ap, binc_res = sc, (w, 5)
        else:
            p.op("dve", lambda e: e.tensor_tensor(out=v3(binc), in0=tot.to_broadcast([128, NCH, C]), in1=sc3, op=ALU.subtract),
                 r=[(w, 5)], w=[(w, 6)])
            p.op("dve", lambda e: e.tensor_tensor(out=binc, in0=binc, in1=lf, op=ALU.add), r=[(w, 6), (w, 4)], w=[(w, 6)])
            binc_ap, binc_res = binc, (w, 6)
        p.op("act", lambda e: e.activation(out=qb[:], in_=binc_ap, func=AF.Exp), r=[binc_res], w=[qb])
        p.op("dve", lambda e: e.tensor_tensor(out=qb[:], in0=qb[:], in1=qa, op=ALU.mult), r=[qb, (w, 0)], w=[qb])
        p.op("dve", lambda e: e.tensor_scalar(out=kb[:], in0=binc_ap, scalar1=-1.0, scalar2=ECLAMP, op0=ALU.mult, op1=ALU.min),
             r=[binc_res], w=[kb])
        p.op("act", lambda e: e.activation(out=kb[:], in_=kb[:], func=AF.Exp), r=[kb], w=[kb])
        p.op("dve", lambda e: e.tensor_tensor(out=kb[:], in0=kb[:], in1=kk, op=ALU.mult), r=[kb, (w, 2)], w=[kb])
        p.op("dve", lambda e: e.tensor_tensor(out=v3(tmp), in0=tot.to_broadcast([128, NCH, C]), in1=v3(binc_ap), op=ALU.subtract),
             r=[(w, 5), binc_res], w=[(w, 7)])
        p.op("act", lambda e: e.activation(out=ke[:], in_=tmp, func=AF.Exp), r=[(w, 7)], w=[ke])
        p.op("dve", lambda e: e.tensor_tensor(out=ke[:], in0=ke[:], in1=kk, op=ALU.mult), r=[ke, (w, 2)], w=[ke])
        p.op("act", lambda e: e.activation(out=ebt[:], in_=tot.rearrange("q c o -> q (c o)"), func=AF.Exp), r=[(w, 5)], w=[ebt])
        ps_s = B[0][0:32, 0:128].rearrange("q (c x) -> q c x", x=32)
        for c in range(NCH):
            p.op("pe", lambda e, c=c: e.matmul(ps_s[:, c, :], lhsT=kb[:, c * C:(c + 1) * C], rhs=qb[:, c * C:(c + 1) * C],
                                               start=True, stop=True), r=[kb, qb], w=[B[0]])
        p.op("dve", lambda e: e.tensor_tensor(out=scm[:], in0=ps_s, in1=mask(d)[:, None, :].to_broadcast([32, NCH, 32]), op=ALU.mult),
             r=[B[0], cst], w=[scm])
        for half in range(2):
            bnk = B[1 + half]
            psx = bnk[0:32, :].rearrange("q (c x) -> q c x", x=256)
            for cc in range(2):
                c = 2 * half + cc
                p.op("pe", lambda e, c=c, cc=cc, psx=psx: e.transpose(out=psx[:, cc, 0:128], in_=ke[:, c * C:(c + 1) * C], identity=ident),
                     r=[ke, cst], w=[bnk])
                p.op("pe", lambda e, c=c, cc=cc, psx=psx: e.transpose(out=psx[:, cc, 128:256], in_=x[:, 2, c * C:(c + 1) * C], identity=ident),
                     r=[(x, 2), cst], w=[bnk])
            p.op("act", lambda e, half=half, psx=psx: e.copy(out=tok[:, 2 * half:2 * half + 2, :], in_=psx), r=[bnk], w=[(tok, half)])
        st, cur = state[(d, h)]
        order = range(NCH) if d == 0 else range(NCH - 1, -1, -1)
        for c in order:
            S0, S1 = st[cur], st[1 - cur]
            psO = B[3 + (c % 2)][:, 0:32]
            bo = B[3 + (c % 2)]
            psS = B[5 + (c % 2)][:, 0:128]
            bs = B[5 + (c % 2)]
            p.op("pe", lambda e, c=c, psO=psO: e.matmul(psO, lhsT=tok[:, c, 128:256], rhs=scm[:, c, :], start=True, stop=False),
                 r=[(tok, c // 2), scm], w=[bo])
            p.op("pe", lambda e, c=c, psO=psO, S0=S0: e.matmul(psO, lhsT=S0[:], rhs=qb[:, c * C:(c + 1) * C], start=False, stop=True),
                 r=[S0, qb], w=[bo])
            p.op("act", lambda e, c=c, psO=psO: e.copy(out=o_t[:, c * C:(c + 1) * C], in_=psO), r=[bo], w=[(o_t, c)])
            p.op("pe", lambda e, c=c, psS=psS: e.matmul(psS, lhsT=tok[:, c, 0:128], rhs=tok[:, c, 128:256], start=True, stop=True),
                 r=[(tok, c // 2)], w=[bs])
            p.op("dve", lambda e, c=c, psS=psS, S0=S0, S1=S1: e.scalar_tensor_tensor(out=S1[:], in0=S0[:], scalar=ebt[:, c:c + 1], in1=psS,
                                                                                      op0=ALU.mult, op1=ALU.add),
                 r=[S0, ebt, bs], w=[S1])
            cur = 1 - cur
        state[(d, h)][1] = cur
        p.dma("sp", OUT[d, h, :, t0:t0 + TB], o_t[:], r=[o_t])

    fwd_blocks = list(range(NB))
    bwd_blocks = list(range(NBC - 1, -1, -1)) + list(range(NB - 1, NBC - 1, -1))
    for s in range(NB):
        for d, blk in ((0, fwd_blocks[s]), (1, bwd_blocks[s])):
            for h in range(NH):
                visit(d, blk, h)
    return p.build()


def l2h_consts():
    cst = np.zeros((128, 128 + 64 + HTB), np.float32)
    cst[:, 0:128] = np.eye(128)
    r = np.arange(32)[:, None]
    cc = np.arange(32)[None, :]
    cst[0:32, 128:160] = (r <= cc)
    cst[0:32, 160:192] = (r >= cc)
    rm = np.ones(HTB, np.float32)
    rm[::HC] = 0.0
    cst[:, 192:192 + HTB] = rm[None, :]
    return cst


RMS_EPS = 1e-6
GN_EPS = 64e-5


def tgroups(NT, g=512):
    out = []
    t = 0
    while t < NT:
        out.append((t, min(g, NT - t)))
        t += g
    return out


def ttiles(NT):
    return [(t, min(128, NT - t)) for t in range(0, NT, 128)]


def build_l3a(NT, n_lat=2048):
    p = Prog()
    xs = p.dram("xs", [NT, D])
    cvec = p.dram("cvec", [2, D])
    ada_w = p.dram("ada_w", [D, D])
    ada_b = p.dram("ada_b", [D])
    HO = p.dram("HO", [2, 1024, NT])
    RO = p.dram("RO", [2, 1024, NT])
    BON = p.dram("BON", [1024, NT])
    GG = p.dram("GG", [1024, NT])
    OG = p.dram("OG", [1024, NT])
    PG = p.dram("PG", [4096, NT])
    PCOL = p.dram("PCOL", [128, 24])
    wba = p.dram("wba", [1024, D])
    wbb = p.dram("wbb", [1024, D])
    wout = p.dram("wout", [D, D])
    CST = p.dram("CST3", [128, 256])
    X1 = p.dram("X1", [NT, D], kind="ExternalOutput")

    B = [p.ps([128, 512], F32, f"bank{i}") for i in range(8)]
    cst = p.sb([128, 256], F32, "cst3")
    p.dma("sp", cst[:], CST, w=[cst])
    ones = cst[:, 0:128]
    blk = cst[:, 128:256]
    pcol = p.sb([128, 24], F32, "pcol")
    p.dma("sp", pcol[:], PCOL, w=[pcol])

    bufA = p.sb([128, 16, NT], BF16, "bufA")
    bufB = p.sb([128, 16, NT], BF16, "bufB")
    wbuf = [p.sb([128, KT, 512], BF16, f"wb{i}") for i in range(2)]
    mod = [p.sb([128, D], F32, f"mod{v}") for v in range(2)]
    emit_adaln(p, cvec, ada_w, ada_b, D, mod, B[0:2], wbuf)

    def mk(shape, name, n=2, dt=F32):
        return [p.sb(shape, dt, f"{name}{i}") for i in range(n)]
    IN = mk([128, 4, 256], "rin")
    WK = mk([128, 3, 256], "rwk")
    cnt = [0]
    tgs = tgroups(NT, 256)

    for ct in range(8):
        for (t0, tn) in tgs:
            k = cnt[0] % 2
            cnt[0] += 1
            i_, w_ = IN[k], WK[k]
            rows = slice(ct * 128, (ct + 1) * 128)
            p.dma("sp", i_[:, 0, :tn], HO[0, rows, t0:t0 + tn], w=[(i_, 0)])
            p.dma("sp", i_[:, 1, :tn], HO[1, rows, t0:t0 + tn], w=[(i_, 1)])
            p.dma("sp", i_[:, 2, :tn], OG[rows, t0:t0 + tn], w=[(i_, 2)])
            o = w_[:, 0, :tn]
            p.op("dve", lambda e, i_=i_, o=o, tn=tn: e.tensor_tensor(out=o, in0=i_[:, 0, :tn], in1=i_[:, 1, :tn], op=ALU.add),
                 r=[(i_, 0), (i_, 1)], w=[(w_, 0)])
            p.op("act", lambda e, w_=w_, o=o, tn=tn: e.activation(out=w_[:, 1, :tn], in_=o, func=AF.Square), r=[(w_, 0)], w=[(w_, 1)])
            ps = B[2 + k]
            p.op("pe", lambda e, ps=ps, w_=w_, tn=tn: e.matmul(ps[:, :tn], lhsT=ones, rhs=w_[:, 1, :tn], start=True, stop=True),
                 r=[cst, (w_, 1)], w=[ps])
            p.op("dve", lambda e, ps=ps, w_=w_, tn=tn: e.tensor_scalar(out=w_[:, 1, :tn], in0=ps[:, :tn], scalar1=RMS_EPS, scalar2=None, op0=ALU.add),
                 r=[ps], w=[(w_, 1)])
            p.op("act", lambda e, w_=w_, tn=tn: e.sqrt(out=w_[:, 1, :tn], in_=w_[:, 1, :tn]), r=[(w_, 1)], w=[(w_, 1)])
            p.op("dve", lambda e, w_=w_, tn=tn: e.reciprocal(out=w_[:, 1, :tn], in_=w_[:, 1, :tn]), r=[(w_, 1)], w=[(w_, 1)])
            p.op("dve", lambda e, w_=w_, o=o, tn=tn: e.tensor_tensor(out=o, in0=o, in1=w_[:, 1, :tn], op=ALU.mult),
                 r=[(w_, 0), (w_, 1)], w=[(w_, 0)])
            p.op("act", lambda e, i_=i_, w_=w_, tn=tn: e.activation(out=w_[:, 2, :tn], in_=i_[:, 2, :tn], func=AF.Silu), r=[(i_, 2)], w=[(w_, 2)])
            p.op("dve", lambda e, w_=w_, o=o, tn=tn, ct=ct, t0=t0: e.scalar_tensor_tensor(
                out=bufA[:, ct, t0:t0 + tn], in0=o, scalar=pcol[:, ct * 3:ct * 3 + 1], in1=w_[:, 2, :tn], op0=ALU.mult, op1=ALU.mult),
                r=[(w_, 0), (w_, 2), pcol], w=[(bufA, ct)])
    for ct in range(8):
        for (t0, tn) in tgs:
            k = cnt[0] % 2
            cnt[0] += 1
            i_, w_ = IN[k], WK[k]
            rows = slice(ct * 128, (ct + 1) * 128)
            p.dma("sp", i_[:, 0, :tn], RO[0, rows, t0:t0 + tn], w=[(i_, 0)])
            p.dma("sp", i_[:, 1, :tn], RO[1, rows, t0:t0 + tn], w=[(i_, 1)])
            p.dma("sp", i_[:, 2, :tn], BON[rows, t0:t0 + tn], w=[(i_, 2)])
            p.dma("sp", i_[:, 3, :tn], GG[rows, t0:t0 + tn], w=[(i_, 3)])
            o = w_[:, 0, :tn]
            p.op("dve", lambda e, i_=i_, o=o, tn=tn: e.tensor_tensor(out=o, in0=i_[:, 0, :tn], in1=i_[:, 1, :tn], op=ALU.add),
                 r=[(i_, 0), (i_, 1)], w=[(w_, 0)])
            ps = B[2 + k]
            p.op("pe", lambda e, ps=ps, o=o, tn=tn: e.matmul(ps[:, :tn], lhsT=blk, rhs=o, start=True, stop=True), r=[cst, (w_, 0)], w=[ps])
            p.op("dve", lambda e, ps=ps, o=o, tn=tn: e.tensor_tensor(out=o, in0=o, in1=ps[:, :tn], op=ALU.subtract), r=[(w_, 0), ps], w=[(w_, 0)])
            p.op("act", lambda e, w_=w_, o=o, tn=tn: e.activation(out=w_[:, 1, :tn], in_=o, func=AF.Square), r=[(w_, 0)], w=[(w_, 1)])
            p.op("pe", lambda e, ps=ps, w_=w_, tn=tn: e.matmul(ps[:, :tn], lhsT=blk, rhs=w_[:, 1, :tn], start=True, stop=True),
                 r=[cst, (w_, 1)], w=[ps])
            p.op("dve", lambda e, ps=ps, w_=w_, tn=tn: e.tensor_scalar(out=w_[:, 1, :tn], in0=ps[:, :tn], scalar1=GN_EPS, scalar2=None, op0=ALU.add),
                 r=[ps], w=[(w_, 1)])
            p.op("act", lambda e, w_=w_, tn=tn: e.sqrt(out=w_[:, 1, :tn], in_=w_[:, 1, :tn]), r=[(w_, 1)], w=[(w_, 1)])
            p.op("dve", lambda e, w_=w_, tn=tn: e.reciprocal(out=w_[:, 1, :tn], in_=w_[:, 1, :tn]), r=[(w_, 1)], w=[(w_, 1)])
            p.op("dve", lambda e, w_=w_, o=o, tn=tn: e.tensor_tensor(out=o, in0=o, in1=w_[:, 1, :tn], op=ALU.mult),
                 r=[(w_, 0), (w_, 1)], w=[(w_, 0)])
            p.op("dve", lambda e, o=o, ct=ct: e.tensor_scalar(out=o, in0=o, scalar1=pcol[:, ct * 3 + 1:ct * 3 + 2],
                                                             scalar2=pcol[:, ct * 3 + 2:ct * 3 + 3], op0=ALU.mult, op1=ALU.add),
                 r=[(w_, 0), pcol], w=[(w_, 0)])
            p.op("dve", lambda e, i_=i_, o=o, tn=tn: e.tensor_tensor(out=o, in0=o, in1=i_[:, 2, :tn], op=ALU.add), r=[(w_, 0), (i_, 2)], w=[(w_, 0)])
            p.op("dve", lambda e, i_=i_, o=o, tn=tn, ct=ct, t0=t0: e.tensor_tensor(out=bufA[:, 8 + ct, t0:t0 + tn], in0=o, in1=i_[:, 3, :tn], op=ALU.mult),
                 r=[(w_, 0), (i_, 3)], w=[(bufA, 8 + ct)])
    for sl in range(4):
        wb = wbuf[sl % 2]
        c0 = sl * 512
        for q in range(2):
            p.dma("pool", wb[:, 4 * q:4 * q + 4, :], wba[:, c0:c0 + 512].rearrange("(ct q) c -> q ct c", q=128)[:, 4 * q:4 * q + 4, :], w=[(wb, q)])
            p.dma("pool", wb[:, 8 + 4 * q:8 + 4 * q + 4, :], wbb[:, c0:c0 + 512].rearrange("(ct q) c -> q ct c", q=128)[:, 4 * q:4 * q + 4, :],
                  w=[(wb, 2 + q)])
        for f4 in range(4):
            ft = sl * 4 + f4
            for (t0, tn) in tgs:
                k = cnt[0] % 2
                cnt[0] += 1
                i_, w_ = IN[k], WK[k]
                psa, psb = B[4 + 2 * k], B[5 + 2 * k]
                for which, ps in ((0, psa), (1, psb)):
                    for ct in range(8):
                        p.op("pe", lambda e, ps=ps, wb=wb, which=which, ct=ct, f4=f4, t0=t0, tn=tn: e.matmul(
                            ps[:, :tn], lhsT=wb[:, which * 8 + ct, f4 * 128:(f4 + 1) * 128], rhs=bufA[:, which * 8 + ct, t0:t0 + tn],
                            start=(ct == 0), stop=(ct == 7)),
                            r=[(wb, which * 2 + ct // 4), (bufA, which * 8 + ct)], w=[ps])
                p.dma("sp", i_[:, 0, :tn], PG[ft * 128:(ft + 1) * 128, t0:t0 + tn], w=[(i_, 0)])
                p.dma("sp", i_[:, 1, :tn], PG[2048 + ft * 128:2048 + (ft + 1) * 128, t0:t0 + tn], w=[(i_, 1)])
                p.op("act", lambda e, i_=i_, tn=tn: e.activation(out=i_[:, 0:2, :tn], in_=i_[:, 0:2, :tn], func=AF.Sigmoid),
                     r=[(i_, 0), (i_, 1)], w=[(i_, 0), (i_, 1)])
                p.op("dve", lambda e, i_=i_, w_=w_, psa=psa, tn=tn: e.tensor_tensor(out=w_[:, 0, :tn], in0=i_[:, 0, :tn], in1=psa[:, :tn], op=ALU.mult),
                     r=[(i_, 0), psa], w=[(w_, 0)])
                p.op("dve", lambda e, i_=i_, w_=w_, psb=psb, tn=tn: e.tensor_tensor(out=w_[:, 1, :tn], in0=i_[:, 1, :tn], in1=psb[:, :tn], op=ALU.mult),
                     r=[(i_, 1), psb], w=[(w_, 1)])
                p.op("pool", lambda e, w_=w_, ft=ft, t0=t0, tn=tn: e.tensor_tensor(out=bufB[:, ft, t0:t0 + tn], in0=w_[:, 0, :tn], in1=w_[:, 1, :tn], op=ALU.add),
                     r=[(w_, 0), (w_, 1)], w=[(bufB, ft)])
    r3 = lambda ap: ap.rearrange("q (a b) -> q a b", a=2)
    for cg in range(4):
        wb = wbuf[cg % 2]
        for q in range(4):
            p.dma("pool", wb[:, 4 * q:4 * q + 4, :], wout[:, cg * 512:(cg + 1) * 512].rearrange("(kt q) c -> q kt c", q=128)[:, 4 * q:4 * q + 4, :],
                  w=[(wb, q)])
        for ti, (t0, rows) in enumerate(ttiles(NT)):
            k = cnt[0] % 2
            cnt[0] += 1
            x_t = WK[k]
            w_ = IN[k]
            v = 0 if t0 < n_lat else 1
            ps = B[2 + k]
            for kt in range(KT):
                p.op("pe", lambda e, ps=ps, wb=wb, kt=kt, t0=t0, rows=rows: e.matmul(ps[:rows, :], lhsT=bufB[:, kt, t0:t0 + rows], rhs=wb[:, kt, :],
                                                                                     start=(kt == 0), stop=(kt == KT - 1)),
                     r=[(bufB, kt), (wb, kt // 4)], w=[ps])
            p.dma("sp", x_t[:rows, 0:2, :], r3(xs[t0:t0 + rows, cg * 512:(cg + 1) * 512]), w=[x_t])
            p.op("dve", lambda e, ps=ps, w_=w_, rows=rows, v=v, cg=cg: e.tensor_tensor(
                out=w_[:rows, 0:2, :], in0=r3(mod[v][:rows, cg * 512:(cg + 1) * 512]), in1=r3(ps[:rows, :]), op=ALU.mult),
                r=[mod[v], ps], w=[w_])
            p.op("pool", lambda e, x_t=x_t, w_=w_, rows=rows: e.tensor_tensor(out=x_t[:rows, 0:2, :], in0=x_t[:rows, 0:2, :], in1=w_[:rows, 0:2, :], op=ALU.add),
                 r=[x_t, w_], w=[x_t])
            p.dma("sp", r3(X1[t0:t0 + rows, cg * 512:(cg + 1) * 512]), x_t[:rows, 0:2, :], r=[x_t])
    return p.build()


def l3a_consts():
    c = np.zeros((128, 256), np.float32)
    c[:, 0:128] = 1.0 / 128
    c[0:64, 128:192] = 1.0 / 64
    c[64:128, 192:256] = 1.0 / 64
    return c


RMS_EPS = 1e-6
NE = 32
ALPHA = 1.702
LIM = 7.0
NEL = 4


def build_l3pre(NT, n_lat=2048):
    p = Prog()
    X1 = p.dram("X1", [NT, D])
    cvec = p.dram("cvec", [2, D])
    ada_w = p.dram("ada_w", [D, 2 * D])
    ada_b = p.dram("ada_b", [2 * D])
    nfg = p.dram("nfg", [D])
    rw = p.dram("rw", [D, NE])
    rb = p.dram("rb", [NE])
    CST = p.dram("CST4", [128, 128])
    H2T = p.dram("H2T", [KT, 128, NT], BF16, kind="ExternalOutput")
    COMB = p.dram("COMB", [NT, NE], kind="ExternalOutput")

    Bk = [p.ps([128, 512], F32, f"bank{i}") for i in range(8)]
    identf = p.sb([128, 128], F32, "identf")
    p.dma("sp", identf[:], CST, w=[identf])
    identb = p.sb([128, 128], BF16, "identb")
    p.op("dve", lambda e: e.tensor_copy(out=identb[:], in_=identf[:]), r=[identf], w=[identb])
    h2T = p.sb([128, KT, NT], BF16, "h2T")
    bigB = p.sb([128, 16384], F32, "bigB")
    wbuf = [p.sb([128, KT, 512], BF16, f"wb{i}") for i in range(2)]
    stg = [p.sb([128, 1024], F32, f"stg{i}") for i in range(2)]
    junk = p.sb([128, D], BF16, "junk")
    tiles = ttiles(NT)
    ntile = len(tiles)
    comb = p.sb([128, ntile, NE], F32, "comb")
    rws = p.sb([128, KT, NE], F32, "rws")
    p.dma("sp", rws[:], rw.rearrange("(kt q) e -> q kt e", q=128), w=[rws])
    rbb = p.sb([128, NE], F32, "rbb")
    p.dma("sp", rbb[:], rb.partition_broadcast(128), w=[rbb])
    sml = [p.sb([128, 64], F32, f"sml{i}") for i in range(2)]

    modv = [T(bigB.h[:, v * 4096:(v + 1) * 4096], "bigB") for v in range(2)]
    shared = {"abbs": [T(stg[i].h[:, 0:512], stg[i].name) for i in range(2)]}
    emit_adaln(p, cvec, ada_w[:, 0:2 * D], ada_b[0:2 * D], 2 * D, modv, Bk[0:2], wbuf, shared=shared)
    gbc = T(bigB.h[:, 8192:10240], "bigB")
    p.dma("sp", gbc[:], nfg.partition_broadcast(128), w=[(bigB, "gbc")])
    for v in range(2):
        p.op("dve", lambda e, v=v: e.scalar_tensor_tensor(out=modv[v][:, D:2 * D], in0=modv[v][:, D:2 * D], scalar=1.0, in1=gbc[:],
                                                           op0=ALU.add, op1=ALU.mult), r=[bigB], w=[bigB])
    xb = [T(bigB.h[:, 10240 + i * 2048:10240 + (i + 1) * 2048], "bigB") for i in range(2)]
    h32 = T(bigB.h[:, 14336:16384], "bigB")
    hb = [junk] * 2
    for ti, (t0, rows) in enumerate(tiles):
        k = ti % 2
        x_t, h_t, s_t = xb[k], hb[k], sml[k]
        v = 0 if t0 < n_lat else 1
        xk = (bigB, f"xb{k}")
        p.dma("sp", x_t[:rows, :], X1[t0:t0 + rows, :], w=[xk])
        p.op("act", lambda e, x_t=x_t, s_t=s_t, rows=rows: e.activation(out=junk[:rows, :], in_=x_t[:rows, :], func=AF.Square, accum_out=s_t[:rows, 0:1]),
             r=[xk], w=[junk, s_t])
        p.op("dve", lambda e, s_t=s_t, rows=rows: e.tensor_scalar(out=s_t[:rows, 1:2], in0=s_t[:rows, 0:1], scalar1=1.0 / D, scalar2=RMS_EPS,
                                                                  op0=ALU.mult, op1=ALU.add), r=[s_t], w=[s_t])
        p.op("act", lambda e, s_t=s_t, rows=rows: e.sqrt(out=s_t[:rows, 1:2], in_=s_t[:rows, 1:2]), r=[s_t], w=[s_t])
        p.op("dve", lambda e, s_t=s_t, rows=rows: e.reciprocal(out=s_t[:rows, 1:2], in_=s_t[:rows, 1:2]), r=[s_t], w=[s_t])
        p.op("dve", lambda e, x_t=x_t, s_t=s_t, rows=rows, v=v: e.scalar_tensor_tensor(
            out=x_t[:rows, :], in0=x_t[:rows, :], scalar=s_t[:rows, 1:2], in1=modv[v][:rows, D:2 * D], op0=ALU.mult, op1=ALU.mult),
            r=[xk, s_t, (bigB, "mod")], w=[xk])
        p.op("dve", lambda e, x_t=x_t, rows=rows, v=v: e.tensor_tensor(out=x_t[:rows, :], in0=x_t[:rows, :], in1=modv[v][:rows, 0:D], op=ALU.add),
             r=[xk, (bigB, "mod")], w=[xk])
        p.op("act", lambda e, x_t=x_t, h_t=h_t, rows=rows: e.copy(out=h_t[:rows, :], in_=x_t[:rows, :]), r=[xk], w=[h_t])
        for q in range(4):
            ps = Bk[2 + (q % 2)]
            psb = ps[:].bitcast(BF16)
            for j in range(4):
                kt = 4 * q + j
                p.op("pe", lambda e, psb=psb, h_t=h_t, kt=kt, j=j, rows=rows: e.transpose(
                    out=psb[:, j * 128:j * 128 + rows], in_=h_t[:rows, kt * 128:(kt + 1) * 128], identity=identb[:rows, :rows]),
                    r=[h_t, identb], w=[ps])
            p.op("act", lambda e, psb=psb, q=q, t0=t0, rows=rows: e.copy(
                out=h2T[:, 4 * q:4 * q + 4, t0:t0 + rows], in_=psb[:, 0:512].rearrange("p (j c) -> p j c", j=4)[:, :, :rows]),
                r=[ps], w=[(h2T, ti)])
        h32v = h32[:].rearrange("p (kt c) -> p kt c", kt=KT)
        for q in range(4):
            ps = Bk[4 + (q % 2)]
            for j in range(4):
                kt = 4 * q + j
                p.op("pe", lambda e, ps=ps, x_t=x_t, kt=kt, j=j, rows=rows: e.transpose(
                    out=ps[:, j * 128:j * 128 + rows], in_=x_t[:rows, kt * 128:(kt + 1) * 128], identity=identf[:rows, :rows]),
                    r=[xk, identf], w=[ps])
            p.op("dve", lambda e, ps=ps, q=q, rows=rows: e.tensor_copy(
                out=h32v[:, 4 * q:4 * q + 4, :rows], in_=ps[:].rearrange("p (j c) -> p j c", j=4)[:, :, :rows]),
                r=[ps], w=[(bigB, "h32")])
        psr = Bk[6]
        for kt in range(KT):
            p.op("pe", lambda e, kt=kt, rows=rows: e.matmul(psr[:rows, 0:NE], lhsT=h32v[:, kt, :rows], rhs=rws[:, kt, :], start=(kt == 0), stop=(kt == KT - 1)),
                 r=[(bigB, "h32"), rws], w=[psr])
        lg, m8, ex = s_t[:rows, 8:40], s_t[:rows, 40:48], s_t[:rows, 2:3]
        sm2 = sml[k]
        p.op("dve", lambda e, lg=lg, rows=rows: e.tensor_tensor(out=lg, in0=psr[:rows, 0:NE], in1=rbb[:rows, :], op=ALU.add), r=[psr, rbb], w=[s_t])
        p.op("dve", lambda e, lg=lg, m8=m8: e.max(out=m8, in_=lg), r=[s_t], w=[s_t])
        cm = comb[:rows, ti, :]
        p.op("dve", lambda e, lg=lg, m8=m8, cm=cm: e.tensor_scalar(out=cm, in0=lg, scalar1=m8[:, 3:4], scalar2=None, op0=ALU.is_ge), r=[s_t], w=[(comb, ti)])
        p.op("dve", lambda e, m8=m8, ex=ex: e.tensor_scalar(out=ex, in0=m8[:, 0:1], scalar1=-1.0, scalar2=None, op0=ALU.mult), r=[s_t], w=[s_t])
        p.op("act", lambda e, lg=lg, ex=ex: e.activation(out=lg, in_=lg, func=AF.Exp, bias=ex), r=[s_t], w=[s_t])
        p.op("dve", lambda e, lg=lg, cm=cm: e.tensor_tensor(out=cm, in0=cm, in1=lg, op=ALU.mult), r=[s_t, (comb, ti)], w=[(comb, ti)])
        p.op("dve", lambda e, cm=cm, ex=ex: e.reduce_sum(out=ex, in_=cm, axis=AX.X), r=[(comb, ti)], w=[s_t])
        p.op("dve", lambda e, ex=ex: e.reciprocal(out=ex, in_=ex), r=[s_t], w=[s_t])
        p.op("dve", lambda e, cm=cm, ex=ex: e.tensor_scalar(out=cm, in0=cm, scalar1=ex, scalar2=None, op0=ALU.mult), r=[s_t, (comb, ti)], w=[(comb, ti)])


    for ti, (t0, rows) in enumerate(tiles):
        p.dma("sp", COMB[t0:t0 + rows, :], comb[:rows, ti, :], r=[(comb, ti)])
    p.dma("sp", H2T.rearrange("kt q t -> q kt t"), h2T[:], r=[h2T])
    return p.build()


def build_l3moe(NTC, n_chunks):
    p = Prog()
    NT = NTC
    H2T = p.dram("H2T", [n_chunks, KT, 128, NTC], BF16)
    CMB = p.dram("CMB", [n_chunks, NTC, NEL])
    W1 = p.dram("W1s", [NEL, D, 4096])
    B1 = p.dram("B1C", [128, NEL, 2, 16])
    W2 = p.dram("W2", [NEL, D, D])
    FACCd = p.dram("FACC", [n_chunks * NTC, D], kind="ExternalOutput")

    Bk = [p.ps([128, 512], F32, f"bank{i}") for i in range(8)]
    h2T = p.sb([128, KT, NT], BF16, "h2T")
    bigB = p.sb([128, 16384], F32, "bigB")
    wbuf = [p.sb([128, KT, 512], BF16, f"wb{i}") for i in range(2)]
    actT = p.sb([128, 16, 512], BF16, "actT")
    stg = [p.sb([128, 1024], F32, f"stg{i}") for i in range(2)]
    junk = p.sb([128, D], BF16, "junk")
    tiles = ttiles(NT)
    ntile = len(tiles)
    combs = [p.sb([128, ntile, NEL], F32, f"comb{i}") for i in range(2)]
    b1c = p.sb([128, NEL, 2, 16], F32, "b1c")
    p.dma("sp", b1c[:], B1, w=[b1c])
    p.op("dve", lambda e: e.tensor_scalar(out=b1c[:, :, 1, :], in0=b1c[:, :, 1, :], scalar1=1.0, scalar2=None, op0=ALU.add), r=[b1c], w=[b1c])
    w2v = bigB.h[:].bitcast(BF16).rearrange("p (jt c) -> p jt c", jt=16)
    tgs = tgroups(NT, 512)
    for ch in range(n_chunks):
        comb = combs[ch % 2]
        FACC = FACCd[ch * NTC:(ch + 1) * NTC, :]
        p.dma("sp", h2T[:], H2T[ch].rearrange("kt q t -> q kt t"), w=[h2T])
        for ti, (t0, rows) in enumerate(tiles):
            p.dma("sp", comb[:rows, ti, :], CMB[ch, t0:t0 + rows, :], w=[(comb, ti)])
        for ex_i in range(NEL):
            for q in range(8):
                p.dma("pool", w2v[:, 2 * q:2 * q + 2, :], W2[ex_i].rearrange("(jt q) c -> q jt c", q=128)[:, 2 * q:2 * q + 2, :], w=[(bigB, ("w2", q))])
            for gi, (g0, gn) in enumerate(tgs):
                gt = [(t0, rows) for (t0, rows) in tiles if g0 <= t0 < g0 + gn]
                for sl in range(8):
                    wb = wbuf[sl % 2]
                    for q in range(4):
                        p.dma("pool", wb[:, 4 * q:4 * q + 4, :], W1[ex_i, :, sl * 512:(sl + 1) * 512].rearrange("(kt q) c -> q kt c", q=128)[:, 4 * q:4 * q + 4, :],
                              w=[(wb, q)])
                    for j2 in range(2):
                        jt = 2 * sl + j2
                        psg, psl = Bk[2 * (jt % 2)], Bk[2 * (jt % 2) + 1]
                        for which, ps in ((0, psg), (1, psl)):
                            for kt in range(KT):
                                p.op("pe", lambda e, ps=ps, wb=wb, which=which, j2=j2, kt=kt, g0=g0, gn=gn: e.matmul(
                                    ps[:, :gn], lhsT=wb[:, kt, j2 * 256 + which:(j2 + 1) * 256:2], rhs=h2T[:, kt, g0:g0 + gn],
                                    start=(kt == 0), stop=(kt == KT - 1)),
                                    r=[(wb, kt // 4)] + [(h2T, ti) for ti, (t0, _) in enumerate(tiles) if g0 <= t0 < g0 + gn], w=[ps])
                        k = jt % 2
                        xg, sgm, xl = stg[k][:, 0:gn], stg[k][:, 512:512 + gn], actT[:, jt, :gn]
                        tmpb = junk[:, k * 1024:k * 1024 + gn]
                        p.op("dve", lambda e, psg=psg, xg=xg, jt=jt, gn=gn, ex_i=ex_i: e.tensor_scalar(out=xg, in0=psg[:, :gn], scalar1=b1c[:, ex_i, 0, jt:jt + 1], scalar2=LIM,
                                                                                                      op0=ALU.add, op1=ALU.min), r=[psg, b1c], w=[(stg[k], 0)])
                        p.op("act", lambda e, xg=xg, sgm=sgm: e.activation(out=sgm, in_=xg, func=AF.Sigmoid, scale=ALPHA), r=[(stg[k], 0)], w=[(stg[k], 1)])
                        p.op("act", lambda e, psl=psl, tmpb=tmpb, jt=jt, gn=gn, ex_i=ex_i: e.activation(out=tmpb, in_=psl[:, :gn], func=AF.Identity,
                                                                                                      bias=b1c[:, ex_i, 1, jt:jt + 1]), r=[psl, b1c], w=[(junk, k)])
                        p.op("pool", lambda e, xg=xg, sgm=sgm: e.tensor_tensor(out=xg, in0=xg, in1=sgm, op=ALU.mult), r=[(stg[k], 0), (stg[k], 1)], w=[(stg[k], 0)])
                        p.op("pool", lambda e, tmpb=tmpb: e.tensor_scalar(out=tmpb, in0=tmpb, scalar1=LIM + 1.0, scalar2=-LIM + 1.0, op0=ALU.min, op1=ALU.max),
                             r=[(junk, k)], w=[(junk, k)])
                        p.op("dve", lambda e, xl=xl, xg=xg, tmpb=tmpb: e.tensor_tensor(out=xl, in0=tmpb, in1=xg, op=ALU.mult), r=[(junk, k), (stg[k], 0)], w=[(actT, jt)])
                for (t0, rows) in gt:
                    ti = t0 // 128
                    for half in range(2):
                        st2 = stg[half]
                        st2k = (stg[half], 2)
                        for c2 in range(2):
                            cg = 2 * half + c2
                            ps = Bk[4 + (cg % 4)]
                            for jt in range(16):
                                p.op("pe", lambda e, ps=ps, jt=jt, t0=t0, g0=g0, rows=rows, cg=cg: e.matmul(
                                    ps[:rows, :], lhsT=actT[:, jt, t0 - g0:t0 - g0 + rows], rhs=w2v[:, jt, cg * 512:(cg + 1) * 512],
                                    start=(jt == 0), stop=(jt == 15)),
                                    r=[(actT, jt), (bigB, ("w2", jt // 2))], w=[ps])
                            p.op("act", lambda e, ps=ps, st2=st2, c2=c2, rows=rows, ti=ti, ex_i=ex_i, comb=comb: e.activation(
                                out=st2[:rows, c2 * 512:(c2 + 1) * 512], in_=ps[:rows, :], func=AF.Copy, scale=comb[:rows, ti, ex_i:ex_i + 1]),
                                r=[ps, (comb, ti)], w=[stg[half]])
                        fk = type("R", (), {"name": f"facc_{ch}_{ti}_{half}"})()
                        if ex_i == 0:
                            p.dma("pool", FACC[t0:t0 + rows, half * 1024:(half + 1) * 1024], st2[:rows, :], r=[stg[half]], w=[fk])
                        else:
                            p.dma("pool", FACC[t0:t0 + rows, half * 1024:(half + 1) * 1024], st2[:rows, :], r=[stg[half]], w=[fk], accum_op=ALU.add)


    return p.build()


def build_l3post(NT, n_lat=2048, final=False, n_parts=8):
    p = Prog()
    X1 = p.dram("X1", [NT, D])
    PARTS = p.dram("PARTS", [n_parts, NT, D])
    COMBd = p.dram("COMB", [NT, NE])
    cvec = p.dram("cvec", [2, D])
    ada_w = p.dram("ada_w", [D, D])
    ada_b = p.dram("ada_b", [D])
    b2 = p.dram("b2", [NE, D])
    fng = p.dram("fng", [D])
    CST = p.dram("CST4", [128, 128])
    XO = p.dram("XO", [NT, D], kind="ExternalOutput")

    Bk = [p.ps([128, 512], F32, f"bank{i}") for i in range(8)]
    identf = p.sb([128, 128], F32, "identf")
    p.dma("sp", identf[:], CST, w=[identf])
    bigB = p.sb([128, 16384], F32, "bigB")
    wbuf = [p.sb([128, KT, 512], BF16, f"wb{i}") for i in range(2)]
    stg = [p.sb([128, 1024], F32, f"stg{i}") for i in range(2)]
    pbuf = [p.sb([128, D], F32, f"pbuf{i}") for i in range(2)]
    junk = p.sb([128, D], BF16, "junk")
    tiles = ttiles(NT)
    ntile = len(tiles)
    comb = p.sb([128, ntile, NE], F32, "comb")
    for ti, (t0, rows) in enumerate(tiles):
        p.dma("sp", comb[:rows, ti, :], COMBd[t0:t0 + rows, :], w=[(comb, ti)])
    sml = [p.sb([128, 64], F32, f"sml{i}") for i in range(2)]
    shared = {}
    modg = [T(bigB.h[:, v * 2048:(v + 1) * 2048], "bigB") for v in range(2)]
    emit_adaln(p, cvec, ada_w, ada_b, D, modg, Bk[0:2], wbuf, shared=shared, wkey="modg")
    fgb = T(bigB.h[:, 4096:6144], "bigB")
    if final:
        p.dma("sp", fgb[:], fng.partition_broadcast(128), w=[(bigB, "fgb")])
    b2s = T(bigB.h[0:32, 14336:16384], "bigB")
    p.dma("sp", b2s[:], b2, w=[(bigB, "b2s")])
    cTs = [T(stg[i].h[0:32, 512:640], stg[i].name) for i in range(2)]
    fa = [T(bigB.h[:, 6144 + i * 2048:6144 + (i + 1) * 2048], "bigB") for i in range(2)]
    xx = [T(bigB.h[:, 10240 + i * 2048:10240 + (i + 1) * 2048], "bigB") for i in range(2)]
    for ti, (t0, rows) in enumerate(tiles):
        k = ti % 2
        f_t, x_t, s_t = fa[k], xx[k], sml[k]
        fk, xk = (bigB, f"fa{k}"), (bigB, f"xx{k}")
        v = 0 if t0 < n_lat else 1
        p.dma("sp", f_t[:rows, :], PARTS[0, t0:t0 + rows, :], w=[fk])
        for j in range(1, n_parts):
            pb_ = pbuf[j % 2]
            p.dma("sp", pb_[:rows, :], PARTS[j, t0:t0 + rows, :], w=[pb_])
            p.op("dve" if j % 2 else "pool", lambda e, f_t=f_t, pb_=pb_, rows=rows: e.tensor_tensor(out=f_t[:rows, :], in0=f_t[:rows, :], in1=pb_[:rows, :], op=ALU.add),
                 r=[fk, pb_], w=[fk])
        p.dma("sp", x_t[:rows, :], X1[t0:t0 + rows, :], w=[xk])
        cT = cTs[k]
        pst = Bk[7]
        p.op("pe", lambda e, ti=ti, rows=rows: e.transpose(out=pst[0:32, :rows], in_=comb[:rows, ti, :], identity=identf[:rows, :rows]),
             r=[(comb, ti), identf], w=[pst])
        p.op("act", lambda e, cT=cT, rows=rows: e.copy(out=cT[:, :rows], in_=pst[0:32, :rows]), r=[pst], w=[cT])
        for cg in range(4):
            ps = Bk[2 + cg % 2]
            p.op("pe", lambda e, ps=ps, cT=cT, rows=rows, cg=cg: e.matmul(ps[:rows, :], lhsT=cT[:, :rows], rhs=b2s[:, cg * 512:(cg + 1) * 512],
                                                                         start=True, stop=True), r=[cT, (bigB, "b2s")], w=[ps])
            p.op("dve", lambda e, ps=ps, f_t=f_t, rows=rows, cg=cg: e.tensor_tensor(out=f_t[:rows, cg * 512:(cg + 1) * 512], in0=f_t[:rows, cg * 512:(cg + 1) * 512],
                                                                                    in1=ps[:rows, :], op=ALU.add), r=[fk, ps], w=[fk])
        p.op("dve", lambda e, f_t=f_t, rows=rows, v=v: e.tensor_tensor(out=f_t[:rows, :], in0=f_t[:rows, :], in1=modg[v][:rows, :], op=ALU.mult),
             r=[fk, (bigB, "modg")], w=[fk])
        p.op("pool", lambda e, f_t=f_t, x_t=x_t, rows=rows: e.tensor_tensor(out=x_t[:rows, :], in0=x_t[:rows, :], in1=f_t[:rows, :], op=ALU.add),
             r=[fk, xk], w=[xk])
        if final:
            p.op("act", lambda e, x_t=x_t, s_t=s_t, rows=rows: e.activation(out=junk[:rows, :], in_=x_t[:rows, :], func=AF.Square, accum_out=s_t[:rows, 0:1]),
                 r=[xk], w=[junk, s_t])
            p.op("dve", lambda e, s_t=s_t, rows=rows: e.tensor_scalar(out=s_t[:rows, 1:2], in0=s_t[:rows, 0:1], scalar1=1.0 / D, scalar2=RMS_EPS,
                                                                      op0=ALU.mult, op1=ALU.add), r=[s_t], w=[s_t])
            p.op("act", lambda e, s_t=s_t, rows=rows: e.sqrt(out=s_t[:rows, 1:2], in_=s_t[:rows, 1:2]), r=[s_t], w=[s_t])
            p.op("dve", lambda e, s_t=s_t, rows=rows: e.reciprocal(out=s_t[:rows, 1:2], in_=s_t[:rows, 1:2]), r=[s_t], w=[s_t])
            p.op("dve", lambda e, x_t=x_t, s_t=s_t, rows=rows: e.scalar_tensor_tensor(out=x_t[:rows, :], in0=x_t[:rows, :], scalar=s_t[:rows, 1:2], in1=fgb[:rows, :],
                                                                                      op0=ALU.mult, op1=ALU.mult), r=[xk, s_t, (bigB, "fgb")], w=[xk])
        p.dma("sp", XO[t0:t0 + rows, :], x_t[:rows, :], r=[xk])
    return p.build()


A_COLS_ = 5120
B_COLS_ = 3488
GRIDW = 64
_PROGS = {}


def _prog(key, fn):
    if key not in _PROGS:
        _PROGS[key] = fn()
    return _PROGS[key]


def _run(nc, in_maps):
    res = run_bass_kernel_spmd(nc, in_maps, core_ids=list(range(len(in_maps))))
    return res.results


def _c(a):
    return np.ascontiguousarray(a, dtype=np.float32)


def _shift_pb(pbT, CTX, SEQ):
    Cn = pbT.shape[0]
    c = np.arange(Cn)
    ctxp = pbT[:, :CTX]
    oc = np.zeros_like(ctxp)
    ev = (c % 2 == 0)
    od = ~ev
    oc[ev, 1:] = ctxp[ev, :-1]
    oc[od, :-1] = ctxp[od, 1:]
    rows = SEQ // GRIDW
    lat = pbT[:, CTX:].reshape(Cn, rows, GRIDW)
    ol = np.zeros_like(lat)
    m = [(c % 4 == i) for i in range(4)]
    ol[m[0], :, 1:] = lat[m[0], :, :-1]
    ol[m[1], :, :-1] = lat[m[1], :, 1:]
    ol[m[2], 1:, :] = lat[m[2], :-1, :]
    ol[m[3], :-1, :] = lat[m[3], 1:, :]
    return np.concatenate([oc, ol.reshape(Cn, SEQ)], axis=1)


def _w1_layout(w1):
    E, Dm, _ = w1.shape
    g = w1[:, :, 0::2].reshape(E, Dm, 8, 256)
    l = w1[:, :, 1::2].reshape(E, Dm, 8, 256)
    return np.ascontiguousarray(np.stack([g, l], 3).reshape(E, Dm, 4096))


def _b1_layout(b1):
    E = b1.shape[0]
    g = b1[:, 0::2].reshape(E, 16, 128)
    l = b1[:, 1::2].reshape(E, 16, 128)
    return np.ascontiguousarray(np.stack([g, l], 1).transpose(3, 0, 1, 2))


def kernel(x, c, ctx, c_ctx, ada_w, ada_b, norm_mix_g, norm_ffn_g, final_norm_g, w_in,
           hgrn_lb_logits, hgrn_norm_g, rwkv_mu, rwkv_w0, rwkv_w_up, rwkv_a0, rwkv_a_up,
           rwkv_g_up, rwkv_k_k, rwkv_k_a, rwkv_r_k, rwkv_ln_w, rwkv_ln_b, w_branch_a,
           w_branch_b, w_out, router_w, router_b, expert_w1, expert_b1, expert_w2, expert_b2):
    x = np.asarray(x, np.float32)
    ctx = np.asarray(ctx, np.float32)
    Bn, SEQ, Dm = x.shape
    CTX = ctx.shape[1]
    DEPTH = ada_w.shape[0]
    NQ = 8 // Bn
    nl, ncx = SEQ // NQ, CTX // NQ
    Tt = CTX + SEQ
    ident = np.eye(128, dtype=np.float32)
    xl = [x[b] for b in range(Bn)]
    xc = [ctx[b] for b in range(Bn)]
    cores = [(b, q) for b in range(Bn) for q in range(NQ)]

    for l in range(DEPTH):
        last = (l == DEPTH - 1)
        cvecs = [_c(np.stack([c[b], c_ctx])) for b in range(Bn)]
        nc1 = _prog(("l1", nl, ncx), lambda: build_l1(nl, ncx))
        xs_core = [_c(np.concatenate([xl[b][q * nl:(q + 1) * nl], xc[b][q * ncx:(q + 1) * ncx]], 0)) for (b, q) in cores]
        adw1, adb1, wl = _c(ada_w[l][:, :2 * Dm]), _c(ada_b[l][:2 * Dm]), _c(w_in[l])
        ims = [dict(xs=xs_core[i], cvec=cvecs[b], ada_w=adw1, ada_b=adb1, norm_g=_c(norm_mix_g[l]), w_in=wl, identd=ident)
               for i, (b, q) in enumerate(cores)]
        r1 = _run(nc1, ims)
        PTb = []
        for b in range(Bn):
            pt = np.empty((r1[0]["PT"].shape[0], Tt), np.float32)
            for q in range(NQ):
                o = r1[b * NQ + q]["PT"]
                pt[:, CTX + q * nl:CTX + (q + 1) * nl] = o[:, :nl]
                pt[:, q * ncx:(q + 1) * ncx] = o[:, nl:]
            PTb.append(pt)
        del r1
        nch = _prog(("l2h", CTX, SEQ, l), lambda: build_l2h(CTX, SEQ, l))
        hconst = l2h_consts()
        ims = []
        for (b, g) in cores:
            pa = PTb[b][:A_COLS_]
            HBm = np.empty((HNH, 4, 128, Tt), np.float32)
            for hh in range(HNH):
                H = g * HNH + hh
                for a, base in enumerate((0, 1024, 2048, 3072)):
                    HBm[hh, a] = pa[base + H * 128:base + (H + 1) * 128]
            lg = hgrn_lb_logits[:, :, g * HNH * 128:(g + 1) * HNH * 128].reshape(2, 2, HNH, 128)
            LBLm = _c(np.transpose(lg, (3, 0, 1, 2)).reshape(128, 2 * 2 * HNH))
            ims.append(dict(HB=HBm, LBL=LBLm, HCST=hconst))
        rh = _run(nch, ims)
        HOb = []
        for b in range(Bn):
            ho = np.empty((2, 1024, Tt), np.float32)
            for g in range(NQ):
                o = rh[b * NQ + g]["HOUT"]
                ho[:, g * HNH * 128:(g + 1) * HNH * 128] = o.reshape(2, HNH * 128, Tt)
            HOb.append(ho)
        del rh
        ncr = _prog(("l2r", CTX, SEQ), lambda: build_l2r(CTX, SEQ))
        rconst = l2r_consts()
        mu = rwkv_mu[l]
        ims = []
        for b in range(Bn):
            pb = PTb[b][A_COLS_:A_COLS_ + B_COLS_]
            pbs = _shift_pb(pb, CTX, SEQ)
            for g in range(NQ):
                RBm = np.empty((NH, 3, 64, Tt), np.float32)
                RBsm = np.empty((NH, 3, 64, Tt), np.float32)
                PCm = np.zeros((64, NPC), np.float32)
                for hh in range(NH):
                    H = g * NH + hh
                    for a in range(3):
                        sl = slice(a * 1024 + H * 64, a * 1024 + (H + 1) * 64)
                        RBm[hh, a] = pb[sl]
                        RBsm[hh, a] = pbs[sl]
                        PCm[:, PCI[("mu", hh, a)]] = mu[sl]
                    PCm[:, PCI[("kk", hh)]] = rwkv_k_k[l][H * 64:(H + 1) * 64]
                    PCm[:, PCI[("ka", hh)]] = rwkv_k_a[l][H * 64:(H + 1) * 64]
                    PCm[:, PCI[("rk", hh)]] = rwkv_r_k[l][H]
                    for d in range(2):
                        PCm[:, PCI[("w0", d, hh)]] = rwkv_w0[l][d][H * 64:(H + 1) * 64]
                        PCm[:, PCI[("a0", d, hh)]] = rwkv_a0[l][d][H * 64:(H + 1) * 64]
                for i in range(4):
                    PCm[:, PCI[("mulr", i)]] = mu[3072 + i * 64:3072 + (i + 1) * 64]
                LRm = _c(pb[3072:3328].reshape(4, 64, Tt))
                LRsm = _c(pbs[3072:3328].reshape(4, 64, Tt))
                MUGm = np.zeros((128, 2), np.float32)
                MUGm[:, 0] = mu[3328:3456]
                MUGm[:32, 1] = mu[3456:3488]
                ch = slice(g * NH * 64, (g + 1) * NH * 64)
                ims.append(dict(RB=RBm, RBs=RBsm, LR=LRm, LRs=LRsm, GD=_c(pb[3328:3488]), GDs=_c(pbs[3328:3488]), PC=PCm, MUG=MUGm,
                                WUP=_c(rwkv_w_up[l][:, :, ch]), AUP=_c(rwkv_a_up[l][:, :, ch]), GUP=_c(rwkv_g_up[l][:, ch]), CST=rconst))
            del pbs
        rr = _run(ncr, ims)
        del ims
        ROb, BONb, GGb = [], [], []
        for b in range(Bn):
            ro = np.empty((2, 1024, Tt), np.float32)
            bo = np.empty((1024, Tt), np.float32)
            gg = np.empty((1024, Tt), np.float32)
            for g in range(NQ):
                r_ = rr[b * NQ + g]
                ch = slice(g * NH * 64, (g + 1) * NH * 64)
                ro[:, ch] = r_["OUT"].reshape(2, NH * 64, Tt)
                bo[ch] = r_["BON"].reshape(NH * 64, Tt)
                gg[ch] = r_["GO"].reshape(NH * 64, Tt)
            ROb.append(ro); BONb.append(bo); GGb.append(gg)
        del rr
        NT = nl if last else nl + ncx
        nca = _prog(("l3a", NT, nl), lambda: build_l3a(NT, nl))
        PCOLm = np.zeros((128, 24), np.float32)
        for ct in range(8):
            PCOLm[:, ct * 3] = hgrn_norm_g[l][ct * 128:(ct + 1) * 128]
            PCOLm[:, ct * 3 + 1] = rwkv_ln_w[l][ct * 128:(ct + 1) * 128]
            PCOLm[:, ct * 3 + 2] = rwkv_ln_b[l][ct * 128:(ct + 1) * 128]
        c3 = l3a_consts()
        adwg, adbg = _c(ada_w[l][:, 2 * Dm:3 * Dm]), _c(ada_b[l][2 * Dm:3 * Dm])
        wba_, wbb_, wo_ = _c(w_branch_a[l]), _c(w_branch_b[l]), _c(w_out[l])
        ims = []
        for i, (b, q) in enumerate(cores):
            cols = np.arange(CTX + q * nl, CTX + (q + 1) * nl)
            if not last:
                cols = np.concatenate([cols, np.arange(q * ncx, (q + 1) * ncx)])
            ims.append(dict(xs=_c(xs_core[i][:NT]), cvec=cvecs[b], ada_w=adwg, ada_b=adbg,
                            HO=_c(HOb[b][:, :, cols]), RO=_c(ROb[b][:, :, cols]), BON=_c(BONb[b][:, cols]), GG=_c(GGb[b][:, cols]),
                            OG=_c(PTb[b][4096:5120][:, cols]), PG=_c(PTb[b][A_COLS_ + B_COLS_:][:, cols]), PCOL=PCOLm,
                            wba=wba_, wbb=wbb_, wout=wo_, CST3=c3))
        ra = _run(nca, ims)
        del ims, PTb, HOb, ROb, BONb, GGb
        ncp = _prog(("l3pre", NT, nl), lambda: build_l3pre(NT, nl))
        adw2, adb2 = _c(ada_w[l][:, 3 * Dm:5 * Dm]), _c(ada_b[l][3 * Dm:5 * Dm])
        ims = [dict(X1=ra[i]["X1"], cvec=cvecs[b], ada_w=adw2, ada_b=adb2, nfg=_c(norm_ffn_g[l]), rw=_c(router_w[l]), rb=_c(router_b[l]), CST4=ident)
               for i, (b, q) in enumerate(cores)]
        rp = _run(ncp, ims)
        del ims
        ncm = _prog(("l3moe", NT, len(cores)), lambda: build_l3moe(NT, len(cores)))
        h2all = np.stack([rp[i]["H2T"] for i in range(len(cores))], 0)
        comball = np.stack([rp[i]["COMB"] for i in range(len(cores))], 0)
        ims = []
        for cix in range(len(cores)):
            es = slice(cix * NEL, (cix + 1) * NEL)
            ims.append(dict(H2T=h2all, CMB=_c(comball[:, :, es]), W1s=_c(expert_w1[l][es]),
                            B1C=_b1_layout(np.asarray(expert_b1[l][es], np.float32)), W2=_c(expert_w2[l][es])))
        rm = _run(ncm, ims)
        del ims, h2all
        nco = _prog(("l3post", NT, nl, last), lambda: build_l3post(NT, nl, last, len(cores)))
        adw3, adb3 = _c(ada_w[l][:, 5 * Dm:6 * Dm]), _c(ada_b[l][5 * Dm:6 * Dm])
        ims = [dict(X1=ra[i]["X1"], PARTS=np.stack([rm[cix]["FACC"][i * NT:(i + 1) * NT] for cix in range(len(cores))], 0), COMB=rp[i]["COMB"],
                    cvec=cvecs[b], ada_w=adw3, ada_b=adb3, b2=_c(expert_b2[l]), fng=_c(final_norm_g), CST4=ident) for i, (b, q) in enumerate(cores)]
        del rm
        rb_ = _run(nco, ims)
        del ims, ra, rp
        xl = [np.concatenate([rb_[b * NQ + q]["XO"][:nl] for q in range(NQ)], 0) for b in range(Bn)]
        if not last:
            xc = [np.concatenate([rb_[b * NQ + q]["XO"][nl:] for q in range(NQ)], 0) for b in range(Bn)]
        del rb_
    return np.stack(xl, 0).astype(np.float32)
```
